# Optimizing a Trainium2 kernel written in Bass

```python
import math
import jax, jax.numpy as jnp
from jax import lax
import numpy as np

D_MODEL = 1024
BATCH = 16
SEQ = 4096
DEPTH = 1

ATTN_HEADS = 8
ATTN_HEAD_DIM = 64
ATTN_W = ATTN_HEADS * ATTN_HEAD_DIM
LRU_W = D_MODEL - ATTN_W
LRU_BLOCKS = 8
LRU_BW = LRU_W // LRU_BLOCKS
LRU_C = 8.0
CONV_W = 4
IN_COLS = 3 * ATTN_W + 2 * LRU_W
Q_BLOCK = 128
PEER_HEADS = 8
PEER_DK = 128
PEER_DH = PEER_DK // 2
N_KEYS = 128
N_EXPERTS = N_KEYS * N_KEYS
PEER_TOPK = 16
PEER_CHUNK = 128
EPS = 1e-6

kernel_name = "hymba_sbattn_rglru_peer_block"


def _rmsnorm(x, g):
    xf = x.astype(jnp.float32)
    y = xf * lax.rsqrt(jnp.mean(xf * xf, axis=-1, keepdims=True) + EPS)
    return y * g.astype(jnp.float32)


def _stick_breaking_attention(q, k, v):
    S = q.shape[2]
    scale = 1.0 / math.sqrt(q.shape[-1])
    outs = []
    for qb in range(S // Q_BLOCK):
        t0 = qb * Q_BLOCK
        t1 = t0 + Q_BLOCK
        qi = q[:, :, t0:t1]
        kp = k[:, :, :t1]
        vp = v[:, :, :t1]
        z = jnp.einsum('bhqd,bhkd->bhqk', qi, kp) * scale
        mask = jnp.arange(t1)[None, :] < jnp.arange(t0, t1)[:, None]
        log_fail = jnp.where(mask, jax.nn.log_sigmoid(-z), 0.0)
        later = lax.cumsum(log_fail, axis=3, reverse=True) - log_fail
        w = jnp.where(mask, jnp.exp(jax.nn.log_sigmoid(z) + later), 0.0)
        outs.append(jnp.einsum('bhqk,bhkd->bhqd', w, vp))
    return jnp.concatenate(outs, axis=2)


def _causal_depthwise_conv(x, w, b):
    S = x.shape[1]
    xp = jnp.pad(x, ((0, 0), (CONV_W - 1, 0), (0, 0)))
    y = b
    for tap in range(CONV_W):
        y = y + xp[:, tap:tap + S, :] * w[tap]
    return y


def _rg_lru(xb, w_rg, b_rg, w_ig, b_ig, lam):
    B, S, W = xb.shape
    xh = xb.reshape(B, S, LRU_BLOCKS, LRU_BW)
    r = jax.nn.sigmoid(jnp.einsum('bshi,hij->bshj', xh, w_rg).reshape(B, S, W) + b_rg)
    i = jax.nn.sigmoid(jnp.einsum('bshi,hij->bshj', xh, w_ig).reshape(B, S, W) + b_ig)
    log_a = -LRU_C * r * jax.nn.softplus(-lam)
    a = jnp.exp(log_a)
    u = jnp.sqrt(-jnp.expm1(2.0 * log_a)) * (i * xb)

    def combine(left, right):
        a1, b1 = left
        a2, b2 = right
        return a1 * a2, a2 * b1 + b2

    _, h = lax.associative_scan(combine, (a, u), axis=1)
    return h


def _peer(h, w_pq, sub_keys1, sub_keys2, expert_u, expert_v):
    B, S, D = h.shape
    tokens = h.reshape(B * S // PEER_CHUNK, PEER_CHUNK, D)

    def chunk_fn(xc):
        T = xc.shape[0]
        q = (xc @ w_pq).reshape(T, PEER_HEADS, PEER_DK)
        s1 = jnp.einsum('thd,hnd->thn', q[..., :PEER_DH], sub_keys1)
        s2 = jnp.einsum('thd,hnd->thn', q[..., PEER_DH:], sub_keys2)
        v1, i1 = lax.top_k(s1, PEER_TOPK)
        v2, i2 = lax.top_k(s2, PEER_TOPK)
        cand = (v1[..., :, None] + v2[..., None, :]).reshape(T, PEER_HEADS, PEER_TOPK * PEER_TOPK)
        cidx = (i1[..., :, None] * N_KEYS + i2[..., None, :]).reshape(T, PEER_HEADS, PEER_TOPK * PEER_TOPK)
        top_s, pos = lax.top_k(cand, PEER_TOPK)
        eidx = jnp.take_along_axis(cidx, pos, axis=-1)
        g = jax.nn.softmax(top_s.astype(jnp.float32), axis=-1)
        u_sel = jnp.take(expert_u, eidx, axis=0)
        act = jax.nn.gelu(jnp.einsum('thkd,td->thk', u_sel, xc))
        v_sel = jnp.take(expert_v, eidx, axis=0)
        return jnp.einsum('thk,thkd->td', g * act, v_sel)

    return lax.map(chunk_fn, tokens).reshape(B, S, D)


def setup_inputs(seed: int = 0) -> dict:
    key = jax.random.key(seed)
    ks = jax.random.split(key, 26)
    f32 = jnp.float32

    def nrm(k, shape, scale):
        return jax.random.normal(k, shape, f32) * scale

    L = DEPTH
    lam_u = jax.random.uniform(ks[12], (L, LRU_W), f32, minval=0.9, maxval=0.999)
    a0 = lam_u ** (1.0 / LRU_C)
    lru_lambda = jnp.log(a0) - jnp.log1p(-a0)
    return {
        "x": nrm(ks[0], (BATCH, SEQ, D_MODEL), 1.0),
        "c": nrm(ks[1], (BATCH, D_MODEL), 1.0),
        "w_mod": nrm(ks[2], (L, D_MODEL, 6 * D_MODEL), D_MODEL ** -0.5),
        "b_mod": nrm(ks[3], (L, 6 * D_MODEL), 0.01),
        "g_norm1": 1.0 + nrm(ks[4], (L, D_MODEL), 0.02),
        "w_in": nrm(ks[5], (L, D_MODEL, IN_COLS), D_MODEL ** -0.5),
        "g_q": 1.0 + nrm(ks[6], (L, ATTN_HEAD_DIM), 0.02),
        "g_k": 1.0 + nrm(ks[7], (L, ATTN_HEAD_DIM), 0.02),
        "conv_w": nrm(ks[8], (L, CONV_W, LRU_W), CONV_W ** -0.5),
        "conv_b": nrm(ks[9], (L, LRU_W), 0.01),
        "w_rg": nrm(ks[10], (L, LRU_BLOCKS, LRU_BW, LRU_BW), LRU_BW ** -0.5),
        "b_rg": nrm(ks[11], (L, LRU_W), 0.01),
        "w_ig": nrm(ks[13], (L, LRU_BLOCKS, LRU_BW, LRU_BW), LRU_BW ** -0.5),
        "b_ig": nrm(ks[14], (L, LRU_W), 0.01),
        "lru_lambda": lru_lambda,
        "g_out_attn": 1.0 + nrm(ks[15], (L, ATTN_W), 0.02),
        "g_out_lru": 1.0 + nrm(ks[16], (L, LRU_W), 0.02),
        "w_out": nrm(ks[17], (L, D_MODEL, D_MODEL), D_MODEL ** -0.5),
        "g_norm2": 1.0 + nrm(ks[18], (L, D_MODEL), 0.02),
        "w_pq": nrm(ks[19], (L, D_MODEL, PEER_HEADS * PEER_DK), D_MODEL ** -0.5),
        "sub_keys1": nrm(ks[20], (L, PEER_HEADS, N_KEYS, PEER_DH), PEER_DH ** -0.5),
        "sub_keys2": nrm(ks[21], (L, PEER_HEADS, N_KEYS, PEER_DH), PEER_DH ** -0.5),
        "expert_u": nrm(ks[22], (L, N_EXPERTS, D_MODEL), D_MODEL ** -0.5),
        "expert_v": nrm(ks[23], (L, N_EXPERTS, D_MODEL), PEER_HEADS ** -0.5),
    }


def reference(x, c, w_mod, b_mod, g_norm1, w_in, g_q, g_k, conv_w, conv_b,
              w_rg, b_rg, w_ig, b_ig, lru_lambda, g_out_attn, g_out_lru, w_out,
              g_norm2, w_pq, sub_keys1, sub_keys2, expert_u, expert_v):
    f32 = jnp.float32
    out_dtype = x.dtype
    B, S, D = x.shape
    h_res = x.astype(f32)
    c_act = jax.nn.silu(c.astype(f32))
    for l in range(DEPTH):
        mod = c_act @ w_mod[l].astype(f32) + b_mod[l].astype(f32)
        shift1, scale1, gate1, shift2, scale2, gate2 = jnp.split(mod, 6, axis=-1)

        h = _rmsnorm(h_res, g_norm1[l]) * (1.0 + scale1[:, None]) + shift1[:, None]
        p = h @ w_in[l].astype(f32)
        q = p[..., 0:ATTN_W]
        k = p[..., ATTN_W:2 * ATTN_W]
        v = p[..., 2 * ATTN_W:3 * ATTN_W]
        lru_x = p[..., 3 * ATTN_W:3 * ATTN_W + LRU_W]
        lru_gate = p[..., 3 * ATTN_W + LRU_W:]

        def heads(t):
            return t.reshape(B, S, ATTN_HEADS, ATTN_HEAD_DIM).transpose(0, 2, 1, 3)

        qh = _rmsnorm(heads(q), g_q[l])
        kh = _rmsnorm(heads(k), g_k[l])
        attn = _stick_breaking_attention(qh, kh, heads(v))
        attn = attn.transpose(0, 2, 1, 3).reshape(B, S, ATTN_W)

        xb = _causal_depthwise_conv(lru_x, conv_w[l].astype(f32), conv_b[l].astype(f32))
        hr = _rg_lru(xb, w_rg[l].astype(f32), b_rg[l].astype(f32), w_ig[l].astype(f32),
                     b_ig[l].astype(f32), lru_lambda[l].astype(f32))
        rec = hr * jax.nn.gelu(lru_gate)

        mixed = jnp.concatenate([_rmsnorm(attn, g_out_attn[l]), _rmsnorm(rec, g_out_lru[l])], axis=-1)
        h_res = h_res + gate1[:, None] * (mixed @ w_out[l].astype(f32))

        h2 = _rmsnorm(h_res, g_norm2[l]) * (1.0 + scale2[:, None]) + shift2[:, None]
        y = _peer(h2, w_pq[l].astype(f32), sub_keys1[l].astype(f32), sub_keys2[l].astype(f32),
                  expert_u[l].astype(f32), expert_v[l].astype(f32))
        h_res = h_res + gate2[:, None] * y
    return h_res.astype(out_dtype)
```

```python
import numpy as np
from contextlib import ExitStack
import concourse.bass as bass
import concourse.mybir as mybir
from concourse.bass_utils import run_bass_kernel_spmd

F32 = mybir.dt.float32
BF16 = mybir.dt.bfloat16
U32 = mybir.dt.uint32
I32 = mybir.dt.int32
AF = mybir.ActivationFunctionType
ALU = mybir.AluOpType
AX = mybir.AxisListType

NCORES = 8
SEQ = 4096
D = 1024
EPS = 1e-6
NT = SEQ // 128
NG = SEQ // 512
GP = 2
NCONST = 128 * 4 + 256


class Sched:
    def __init__(self, nc, es):
        self.nc = nc
        self.engs = {"pe": nc.tensor, "act": nc.scalar, "dve": nc.vector, "pool": nc.gpsimd, "sp": nc.sync}
        self.sem = {k: es.enter_context(nc.semaphore("s_" + k)) for k in self.engs}
        self.cnt = {k: 0 for k in self.engs}
        self.seen = {k: {} for k in self.engs}
        self.res = {}
        self.dsem = {}
        self.dsem_by_id = {}
        self.es = es

    def _need(self, reads, writes):
        need = []
        for r in reads:
            st = self.res.get(r)
            if st and st["w"]:
                need.append(st["w"])
        for w in writes:
            st = self.res.get(w)
            if st:
                if st["w"]:
                    need.append(st["w"])
                need.extend(st["r"].values())
        return need

    def _wait(self, eng, need):
        best = {}
        for (s, v, src) in need:
            if src == "pe" and eng == "pe":
                continue
            k = id(s)
            if k not in best or best[k][1] < v:
                best[k] = (s, v)
        for k, (s, v) in best.items():
            if k in self.dsem_by_id:
                v = self.dsem_by_id[k][1]
            if self.seen[eng].get(k, -1) >= v:
                continue
            self.engs[eng].wait_ge(s, v)
            self.seen[eng][k] = v

    def _upd(self, tok, reads, writes):
        for r in reads:
            st = self.res.setdefault(r, {"w": None, "r": {}})
            k = id(tok[0])
            if k not in st["r"] or st["r"][k][1] < tok[1]:
                st["r"][k] = tok
        for w in writes:
            self.res[w] = {"w": tok, "r": {}}

    def op(self, eng, fn, reads=(), writes=()):
        self._wait(eng, self._need(reads, writes))
        ins = fn(self.engs[eng])
        self.cnt[eng] += 1
        ins.then_inc(self.sem[eng], 1)
        self._upd((self.sem[eng], self.cnt[eng], eng), reads, writes)
        return ins

    def dma(self, eng, group, fn, reads=(), writes=(), after=None):
        if group not in self.dsem:
            self.dsem[group] = [self.es.enter_context(self.nc.semaphore("d_" + str(group))), 0]
            self.dsem_by_id[id(self.dsem[group][0])] = self.dsem[group]
        if after is None:
            self._wait(eng, self._need(reads, writes))
        else:
            self._wait(eng, self._need(reads, ()) + list(after))
        ins = fn(self.engs[eng])
        d = self.dsem[group]
        d[1] += 16
        ins.then_inc(d[0], 16)
        self._upd((d[0], d[1], "dma"), reads, writes)
        return ins

    def barrier(self, engs=None):
        allv = [(self.sem[k], self.cnt[k], k + "_b") for k in self.engs if self.cnt[k] > 0]
        allv += [(d[0], d[1], "dma") for d in self.dsem.values() if d[1] > 0]
        for e in (engs or self.engs):
            self._wait(e, allv)


class _Stop(Exception):
    pass


def build_nc(nseq=2, nt_de=NT, do_peer=True, dbg=False, stop=None):
    try:
        return _build_nc(nseq, nt_de, do_peer, dbg, stop)
    except _Stop as e:
        return e.args[0]


def _build_nc(nseq, nt_de, do_peer, dbg, stop):
    nc = bass.Bass("TRN2", target_bir_lowering=False)
    dram = lambda n, s, dt=F32, kind="ExternalInput": nc.dram_tensor(n, list(s), dt, kind=kind).ap()
    x = dram("x", [2, SEQ, D])
    cT = dram("cT", [128, 16])
    w_mod = dram("w_mod", [D, 6 * D])
    b_mod = dram("b_mod", [1, 6 * D])
    g1 = dram("g1", [1, D])
    g2 = dram("g2", [1, D])
    w_in = dram("w_in", [D, 2560])
    gqk_d = dram("gqk", [128, 2])
    lrup_d = dram("lrup", [128, 32])
    wg_d = dram("wg_bd", [128, 8 * 128])
    gout_d = dram("gout", [128, 8])
    w_out = dram("w_out", [D, D])
    w_pq = dram("w_pq", [D, D])
    skT_d = dram("skT", [128, 8 * 128])
    eu = dram("expert_u", [16384, D])
    ev = dram("expert_v", [16384, D])
    consts_d = dram("consts", [128, NCONST])
    masks_d = dram("masks", [128, 2048])
    out = dram("out", [2, SEQ, D], kind="ExternalOutput")
    am_s = dram("am_s", [SEQ, D], kind="ExternalOutput" if dbg else "Internal")
    am_w = am_s.rearrange("(t p) (k j) -> p t k j", p=128, k=8)
    uvb = dram("uvb", [16384, 2 * D], BF16, kind="Internal")
    if dbg:
        dbg_x1 = dram("dbg_x1", [SEQ, D], kind="ExternalOutput")
        dbg_i = dram("dbg_i", [128, 128], I32, kind="ExternalOutput")
        dbg_g = dram("dbg_g", [128, 128], kind="ExternalOutput")
        dbg_a = dram("dbg_a", [128, 128], kind="ExternalOutput")

    with ExitStack() as es:
        S = Sched(nc, es)
        banks = [es.enter_context(nc.psum_tensor(f"bank{i}", [128, 512], F32)) for i in range(8)]
        BK = lambda i: ("bank", i)

        uid = [0]

        def T(stack, name, shape, dt=F32):
            uid[0] += 1
            return stack.enter_context(nc.sbuf_tensor(f"{name}_{uid[0]}", list(shape), dt))

        def stop_at(tag):
            if stop == tag:
                S.barrier()
                raise _Stop(nc)

        cst = T(es, "cst", [128, NCONST])
        identb = T(es, "identb", [128, 128], BF16)
        mnegb = T(es, "mnegb", [128, 128], BF16)
        negonesb = T(es, "negonesb", [128, 128], BF16)
        maskb = T(es, "maskb", [128, 4, 512], BF16)
        sc = T(es, "sc", [128, 8, 2])
        gqk = T(es, "gqk_t", [128, 2])
        lrup = T(es, "lrup_t", [128, 40])
        wg = T(es, "wg_t", [128, 8, 128])
        gout = T(es, "gout_t", [128, 8])
        mod = T(es, "mod", [128, 4 * D])
        ident_f = cst[:, 0:128]
        negones_f = cst[:, 256:384]
        blockones_f = cst[:, 384:512]
        iota_ka = cst[:, 512:768]

        S.dma("sp", "c0", lambda e: e.dma_start(out=cst[:], in_=consts_d), writes=["cst"])
        S.dma("sp", "c0", lambda e: e.dma_start(out=sc[:], in_=cT.rearrange("p (k b) -> p k b", b=2)), writes=["sc"])
        S.dma("sp", "c0", lambda e: e.dma_start(out=gqk[:], in_=gqk_d), writes=["gqk"])
        S.dma("sp", "c0", lambda e: e.dma_start(out=lrup[:, 0:32], in_=lrup_d), writes=["lrup"])
        S.dma("sp", "c0", lambda e: e.dma_start(out=wg[:], in_=wg_d.rearrange("p (c j) -> p c j", j=128)), writes=["wg"])
        S.dma("sp", "c0", lambda e: e.dma_start(out=gout[:], in_=gout_d), writes=["gout"])
        with ExitStack() as ph:
            mst = T(ph, "mst", [128, 2048])
            S.dma("sp", "c0", lambda e: e.dma_start(out=mst[:], in_=masks_d), writes=["mst"])
            S.barrier()
            S.op("dve", lambda e: e.tensor_copy(out=maskb[:], in_=mst[:].rearrange("p (j t) -> p j t", j=4)),
                 reads=["mst"], writes=["maskb"])
            S.barrier()
        S.op("dve", lambda e: e.tensor_copy(out=identb[:], in_=cst[:, 0:128]), reads=["cst"], writes=["identb"])
        S.op("dve", lambda e: e.tensor_copy(out=mnegb[:], in_=cst[:, 128:256]), reads=["cst"], writes=["mnegb"])
        S.op("dve", lambda e: e.tensor_copy(out=negonesb[:], in_=cst[:, 256:384]), reads=["cst"], writes=["negonesb"])
        S.op("act", lambda e: e.activation(out=sc[:], in_=sc[:], func=AF.Silu), reads=["sc"], writes=["sc"])
        S.op("dve", lambda e: e.tensor_scalar(out=gqk[:, 0:1], in0=gqk[:, 0:1], scalar1=0.125, scalar2=None, op0=ALU.mult),
             reads=["gqk"], writes=["gqk"])
        S.op("act", lambda e: e.activation(out=lrup[:, 32:36], in_=lrup[:, 28:32], func=AF.Exp, scale=-1.0), reads=["lrup"], writes=["lrup"])
        S.op("act", lambda e: e.activation(out=lrup[:, 32:36], in_=lrup[:, 32:36], func=AF.Ln, bias=1.0), reads=["lrup"], writes=["lrup"])
        S.op("dve", lambda e: e.tensor_scalar(out=lrup[:, 36:40], in0=lrup[:, 32:36], scalar1=-16.0, scalar2=None, op0=ALU.mult),
             reads=["lrup"], writes=["lrup"])
        S.op("dve", lambda e: e.tensor_scalar(out=lrup[:, 32:36], in0=lrup[:, 32:36], scalar1=-8.0, scalar2=None, op0=ALU.mult),
             reads=["lrup"], writes=["lrup"])

        for b in range(nseq):
          with ExitStack() as sq:
            modA = T(sq, "modA", [128, 2 * D])
            with ExitStack() as ph:
                screp = T(ph, "screp", [128, 8, 128])
                wm = [T(ph, f"wm{i}", [128, 8, 512]) for i in range(2)]
                gbc = T(ph, "gbc", [128, D])
                onesr = T(ph, "onesr", [1, 128])
                bmod = [T(ph, f"bmod{i}", [1, 512]) for i in range(2)]
                S.op("dve", lambda e: e.memset(onesr[:], 1.0), writes=["onesr"])
                S.op("dve", lambda e: e.tensor_copy(out=screp[:], in_=sc[:, :, b:b + 1].to_broadcast([128, 8, 128])),
                     reads=["sc"], writes=["screp"])
                wmv = w_mod.rearrange("(kc p) n -> p kc n", p=128)
                for nb in range(12):
                    sl = nb % 2
                    S.dma("sp", f"bm{sl}", lambda e: e.dma_start(out=bmod[sl][:], in_=b_mod[0:1, nb * 512:(nb + 1) * 512]),
                          writes=[f"bmod{sl}"])
                    S.dma("sp", f"wm{sl}", lambda e: e.dma_start(out=wm[sl][:], in_=wmv[:, :, nb * 512:(nb + 1) * 512]),
                          writes=[f"wm{sl}"])
                    bk = nb % 2
                    for kc in range(8):
                        S.op("pe", lambda e: e.matmul(banks[bk][:], lhsT=screp[:, kc, :], rhs=wm[sl][:, kc, :],
                                                      start=(kc == 0), stop=False),
                             reads=["screp", f"wm{sl}"], writes=[BK(bk)])
                    S.op("pe", lambda e: e.matmul(banks[bk][:], lhsT=onesr[:], rhs=bmod[sl][:],
                                                  start=False, stop=True),
                         reads=["onesr", f"bmod{sl}"], writes=[BK(bk)])
                    mdst = modA[:, nb * 512:(nb + 1) * 512] if nb < 4 else mod[:, (nb - 4) * 512:(nb - 3) * 512]
                    S.op("act", lambda e: e.activation(out=mdst, in_=banks[bk][:], func=AF.Identity),
                         reads=[BK(bk)], writes=["mod"])
                for (gd, mt, c0) in ((g1, modA, 1024), (g2, mod, 2048)):
                    S.dma("sp", "gbc", lambda e: e.dma_start(out=gbc[:], in_=gd.to_broadcast([128, D])), writes=["gbc"])
                    S.op("dve", lambda e: e.scalar_tensor_tensor(out=mt[:, c0:c0 + D], in0=mt[:, c0:c0 + D], scalar=1.0,
                                                                 in1=gbc[:], op0=ALU.add, op1=ALU.mult),
                         reads=["gbc", "mod"], writes=["mod"])
                S.barrier()

            stop_at("M")
            with ExitStack() as ph:
                winb = T(ph, "winb", [128, 8, 2560], BF16)
                hT = T(ph, "hT", [128, 8, SEQ], BF16)
                with ExitStack() as ph2:
                    wst = [T(ph2, f"wst{i}", [128, 2560]) for i in range(2)]
                    for kc in range(8):
                        sl = kc % 2
                        S.dma("sp", f"wst{sl}", lambda e: e.dma_start(out=wst[sl][:], in_=w_in[kc * 128:(kc + 1) * 128, :]),
                              writes=[f"wst{sl}"])
                        eng = "dve" if kc % 2 == 0 else "pool"
                        S.op(eng, lambda e: e.tensor_copy(out=winb[:, kc, :], in_=wst[sl][:]), reads=[f"wst{sl}"], writes=["winb"])
                    S.barrier()
                with ExitStack() as ph2:
                    xt = [T(ph2, f"xt{i}", [128, D]) for i in range(2)]
                    hn = [T(ph2, f"hn{i}", [128, D]) for i in range(2)]
                    hb = [T(ph2, f"hb{i}", [128, D], BF16) for i in range(2)]
                    junk = T(ph2, "junk", [128, D], BF16)
                    ss = [T(ph2, f"ss{i}", [128, 4]) for i in range(2)]
                    def a1(tt):
                        sl = tt % 2
                        S.dma("sp", f"x{sl}", lambda e: e.dma_start(out=xt[sl][:], in_=x[b, tt * 128:(tt + 1) * 128, :]),
                              writes=[f"xt{sl}"])
                        S.op("act", lambda e: e.activation(out=junk[:], in_=xt[sl][:], func=AF.Square, accum_out=ss[sl][:, 0:1]),
                             reads=[f"xt{sl}"], writes=["junk", f"ss{sl}"])
                        S.op("act", lambda e: e.activation(out=ss[sl][:, 1:2], in_=ss[sl][:, 0:1], func=AF.Sqrt, scale=1.0 / D, bias=EPS),
                             reads=[f"ss{sl}"], writes=[f"ss{sl}"])
                        S.op("dve", lambda e: e.reciprocal(out=ss[sl][:, 2:3], in_=ss[sl][:, 1:2]), reads=[f"ss{sl}"], writes=[f"ss{sl}"])
                        S.op("dve", lambda e: e.scalar_tensor_tensor(out=hn[sl][:], in0=xt[sl][:], scalar=ss[sl][:, 2:3],
                                                                     in1=modA[:, 1024:2048], op0=ALU.mult, op1=ALU.mult),
                             reads=[f"xt{sl}", f"ss{sl}", "mod"], writes=[f"hn{sl}"])
                        S.op("pool", lambda e: e.tensor_tensor(out=hb[sl][:], in0=hn[sl][:], in1=modA[:, 0:1024], op=ALU.add),
                             reads=[f"hn{sl}", "mod"], writes=[f"hb{sl}"])

                    def a2(tt):
                        sl = tt % 2
                        bk = 6 + sl
                        pv = banks[bk][:].bitcast(BF16)
                        for kc in range(8):
                            S.op("pe", lambda e: e.transpose(out=pv[:, kc * 128:(kc + 1) * 128], in_=hb[sl][:, kc * 128:(kc + 1) * 128],
                                                             identity=identb[:]),
                                 reads=[f"hb{sl}", "identb"], writes=[BK(bk)])
                        S.op("act", lambda e: e.activation(out=hT[:, :, tt * 128:(tt + 1) * 128],
                                                           in_=pv.rearrange("p (k j) -> p k j", k=8), func=AF.Identity),
                             reads=[BK(bk)], writes=[("hT", tt // 4)])

                    for tt in range(NT + 1):
                        if tt < NT:
                            a1(tt)
                        if tt >= 1:
                            a2(tt - 1)
                    S.barrier()

                stop_at("A")

                def proj(col0, g, bk):
                    for kc in range(8):
                        S.op("pe", lambda e: e.matmul(banks[bk][:], lhsT=winb[:, kc, col0:col0 + 128],
                                                      rhs=hT[:, kc, g * 512:(g + 1) * 512], start=(kc == 0), stop=(kc == 7)),
                             reads=["winb", ("hT", g)], writes=[BK(bk)])

                with ExitStack() as ph2:
                    kT = T(ph2, "kT", [128, SEQ], BF16)
                    qT = T(ph2, "qT", [128, SEQ], BF16)
                    V = T(ph2, "V", [128, NT, 128], BF16)
                    sqs = [T(ph2, f"sq{i}", [128, 512]) for i in range(2)]
                    rss = [T(ph2, f"rs{i}", [128, 512]) for i in range(2)]
                    NSB = 3
                    E = [T(ph2, f"E{i}", [128, 512]) for i in range(NSB)]
                    L = [T(ph2, f"L{i}", [128, 512], BF16) for i in range(NSB)]
                    W = [T(ph2, f"W{i}", [128, 512], BF16) for i in range(NSB)]
                    cum = [T(ph2, f"cum{i}", [128, 512], BF16) for i in range(2)]
                    osb = [T(ph2, f"osb{i}", [64, 512]) for i in range(2)]
                    pg = None
                    if b == 0:
                        cin = [T(ph2, f"cin{i}", [128, D]) for i in range(2)]
                        cout = [T(ph2, f"cout{i}", [128, D], BF16) for i in range(2)]

                        def prepass():
                            n = 0
                            for ti, src in enumerate((eu, ev)):
                                for ch in range(128):
                                    sl = n % 2
                                    n += 1
                                    S.dma("sp", f"cin{sl}", lambda e: e.dma_start(out=cin[sl][:], in_=src[ch * 128:(ch + 1) * 128, :]),
                                          writes=[f"cin{sl}"])
                                    S.op("pool", lambda e: e.tensor_copy(out=cout[sl][:], in_=cin[sl][:]), reads=[f"cin{sl}"], writes=[f"cout{sl}"])
                                    S.dma("pool", f"cst{sl}", lambda e: e.dma_start(out=uvb[ch * 128:(ch + 1) * 128, ti * D:(ti + 1) * D], in_=cout[sl][:]),
                                          reads=[f"cout{sl}"], writes=["uvb"])
                                    yield
                        pg = prepass()
                    for hp in range(4):
                        for (dst, dname, col0, gcol) in ((kT, "kT", 512 + hp * 128, 1), (qT, "qT", hp * 128, 0)):
                            for g in range(NG):
                                bk = 6 + (g % 2)
                                sb = 4 + (g % 2)
                                sq, rs = sqs[g % 2], rss[g % 2]
                                nsq, nrs = f"sq{g % 2}", f"rs{g % 2}"
                                proj(col0, g, bk)
                                S.op("act", lambda e: e.activation(out=sq[:], in_=banks[bk][:], func=AF.Square),
                                     reads=[BK(bk)], writes=[nsq])
                                S.op("pe", lambda e: e.matmul(banks[sb][:], lhsT=blockones_f, rhs=sq[:], start=True, stop=True),
                                     reads=[nsq, "cst"], writes=[BK(sb)])
                                S.op("act", lambda e: e.activation(out=rs[:], in_=banks[sb][:], func=AF.Sqrt, scale=1.0 / 64, bias=EPS),
                                     reads=[BK(sb)], writes=[nrs])
                                S.op("dve", lambda e: e.reciprocal(out=rs[:], in_=rs[:]), reads=[nrs], writes=[nrs])
                                S.op("dve", lambda e: e.scalar_tensor_tensor(out=dst[:, g * 512:(g + 1) * 512], in0=banks[bk][:],
                                                                             scalar=gqk[:, gcol:gcol + 1], in1=rs[:],
                                                                             op0=ALU.mult, op1=ALU.mult),
                                     reads=[BK(bk), "gqk", nrs], writes=[(dname, g)])
                        for tb4 in range(NT // 4):
                            bk = 6 + (tb4 % 2)
                            for q4 in range(4):
                                tb = tb4 * 4 + q4
                                for kc in range(8):
                                    S.op("pe", lambda e: e.matmul(banks[bk][:, q4 * 128:(q4 + 1) * 128],
                                                                  lhsT=hT[:, kc, tb * 128:(tb + 1) * 128],
                                                                  rhs=winb[:, kc, 1024 + hp * 128:1024 + (hp + 1) * 128],
                                                                  start=(kc == 0), stop=(kc == 7)),
                                         reads=["winb", ("hT", tb // 4)], writes=[BK(bk)])
                            S.op("dve", lambda e: e.tensor_copy(out=V[:, tb4 * 4:(tb4 + 1) * 4, :],
                                                                in_=banks[bk][:].rearrange("p (q j) -> p q j", q=4)),
                                 reads=[BK(bk)], writes=[("V", tb4)])
                        pairs = []
                        for h2 in range(2):
                            for g in range(NG):
                                for idx, kb in enumerate(range(4 * g + 3, -1, -1)):
                                    pairs.append((h2, g, kb, idx, idx == 0, kb == 0, kb - 4 * g, h2 * NG + g))

                        def s1(n):
                            h2, g, kb, idx, first, last, j, run = pairs[n]
                            sl = n % NSB
                            ba = n % 2
                            pb = 64 * h2
                            S.op("pe", lambda e: e.matmul(banks[ba][:], lhsT=kT[pb:pb + 64, kb * 128:(kb + 1) * 128],
                                                          rhs=qT[pb:pb + 64, g * 512:(g + 1) * 512], start=True, stop=True),
                                 reads=[("kT", kb // 4), ("qT", g)], writes=[BK(ba)])
                            S.op("act", lambda e: e.activation(out=E[sl][:], in_=banks[ba][:], func=AF.Exp),
                                 reads=[BK(ba)], writes=[f"E{sl}"])
                            S.op("act", lambda e: e.activation(out=L[sl][:], in_=E[sl][:], func=AF.Ln, bias=1.0),
                                 reads=[f"E{sl}"], writes=[f"L{sl}"])
                            if j >= 0:
                                S.op("dve", lambda e: e.tensor_tensor(out=L[sl][:], in0=L[sl][:], in1=maskb[:, j, :], op=ALU.mult),
                                     reads=[f"L{sl}", "maskb"], writes=[f"L{sl}"])

                        def s2(n):
                            h2, g, kb, idx, first, last, j, run = pairs[n]
                            sl = n % NSB
                            bk = 2 + n % 2
                            pb = 64 * h2
                            S.op("pe", lambda e: e.matmul(banks[bk][:], lhsT=kT[pb:pb + 64, kb * 128:(kb + 1) * 128],
                                                          rhs=qT[pb:pb + 64, g * 512:(g + 1) * 512], start=True, stop=False),
                                 reads=[("kT", kb // 4), ("qT", g)], writes=[BK(bk)])
                            S.op("pe", lambda e: e.matmul(banks[bk][:], lhsT=mnegb[:], rhs=L[sl][:], start=False, stop=first),
                                 reads=["mnegb", f"L{sl}"], writes=[BK(bk)])
                            if not first:
                                S.op("pe", lambda e: e.matmul(banks[bk][:], lhsT=negonesb[:], rhs=cum[idx % 2][:], start=False, stop=True),
                                     reads=["negonesb", f"cum{idx % 2}"], writes=[BK(bk)])
                            S.op("act", lambda e: e.activation(out=W[sl][:], in_=banks[bk][:], func=AF.Exp),
                                 reads=[BK(bk)], writes=[f"W{sl}"])
                            if j >= 0:
                                S.op("dve", lambda e: e.tensor_tensor(out=W[sl][:], in0=W[sl][:], in1=maskb[:, j, :], op=ALU.mult),
                                     reads=[f"W{sl}", "maskb"], writes=[f"W{sl}"])
                            if not last:
                                nx = (idx + 1) % 2
                                if first:
                                    S.op("dve", lambda e: e.tensor_copy(out=cum[nx][:], in_=L[sl][:]),
                                         reads=[f"L{sl}"], writes=[f"cum{nx}"])
                                else:
                                    S.op("dve", lambda e: e.tensor_tensor(out=cum[nx][:], in0=cum[idx % 2][:], in1=L[sl][:], op=ALU.add),
                                         reads=[f"L{sl}", f"cum{idx % 2}"], writes=[f"cum{nx}"])

                        def s3(n):
                            h2, g, kb, idx, first, last, j, run = pairs[n]
                            sl = n % NSB
                            ob = 4 + (run % 2)
                            S.op("pe", lambda e: e.matmul(banks[ob][0:64, :], lhsT=V[:, kb, h2 * 64:(h2 + 1) * 64], rhs=W[sl][:],
                                                          start=first, stop=last),
                                 reads=[("V", kb // 4), f"W{sl}"], writes=[BK(ob)])
                            if last:
                                o = run % 2
                                S.op("dve", lambda e: e.tensor_copy(out=osb[o][:], in_=banks[ob][0:64, :]),
                                     reads=[BK(ob)], writes=[f"osb{o}"])
                                r0 = (hp * 2 + h2) * 64
                                S.dma("sp", f"ost{o}", lambda e: e.dma_start(out=am_w[64 * h2:64 * h2 + 64, 4 * g:4 * g + 4, hp, :],
                                                                             in_=osb[o][:].rearrange("p (q j) -> p q j", q=4)),
                                      reads=[f"osb{o}"], writes=["attn_s"])

                        NP = len(pairs)
                        for m in range(NP + 2):
                            if pg is not None and m % 4 == 0:
                                next(pg, None)
                            if m < NP:
                                s1(m)
                            if 0 <= m - 1 < NP:
                                s2(m - 1)
                            if 0 <= m - 2 < NP:
                                s3(m - 2)
                    if pg is not None:
                        for _ in pg:
                            pass
                    S.barrier()

                stop_at("B")
                with ExitStack() as ph2:
                    def mk(k):
                        d = dict(lx=[T(ph2, f"lx{k}{i}", [128, 3 + 512]) for i in range(2)],
                                 hh=[T(ph2, f"hh{k}{i}", [128, 512]) for i in range(2)],
                                 rec=[T(ph2, f"rec{k}{i}", [128, 512]) for i in range(2)])
                        for nm in ("xb", "r_", "i_", "a_", "u_", "gg"):
                            d[nm] = T(ph2, f"{nm}{k}", [128, 512])
                        return d
                    cb = [mk(0), mk(1)]

                    def lru_chain(c, k):
                        B_ = cb[k]
                        lx, hh, rec = B_["lx"], B_["hh"], B_["rec"]
                        xb_, r_, i_, a_, u_, gg = B_["xb"], B_["r_"], B_["i_"], B_["a_"], B_["u_"], B_["gg"]
                        b0 = 4 * k
                        N = lambda nm: f"{nm}{k}"
                        for g in range(NG):
                            sl = g % 2
                            proj(1536 + c * 128, g, b0)
                            yield
                            if g == 0:
                                S.op("dve", lambda e: e.memset(lx[sl][:, 0:3], 0.0), writes=[N(f"lx{sl}")])
                            S.op("act", lambda e: e.activation(out=lx[sl][:, 3:515], in_=banks[b0][:], func=AF.Identity),
                                 reads=[BK(b0)], writes=[N(f"lx{sl}")])
                            yield
                            if g + 1 < NG:
                                S.op("dve", lambda e: e.tensor_copy(out=lx[1 - sl][:, 0:3], in_=lx[sl][:, 512:515]),
                                     reads=[N(f"lx{sl}")], writes=[N(f"lx{1 - sl}")])
                            S.op("dve", lambda e: e.tensor_scalar(out=xb_[:], in0=lx[sl][:, 0:512], scalar1=lrup[:, c * 4:c * 4 + 1],
                                                                  scalar2=lrup[:, 16 + c:17 + c], op0=ALU.mult, op1=ALU.add),
                                 reads=[N(f"lx{sl}"), "lrup"], writes=[N("xb")])
                            yield
                            for kk in range(1, 4):
                                S.op("dve", lambda e: e.scalar_tensor_tensor(out=xb_[:], in0=lx[sl][:, kk:kk + 512],
                                                                             scalar=lrup[:, c * 4 + kk:c * 4 + kk + 1], in1=xb_[:],
                                                                             op0=ALU.mult, op1=ALU.add),
                                     reads=[N(f"lx{sl}"), "lrup", N("xb")], writes=[N("xb")])
                                yield
                            S.op("pe", lambda e: e.matmul(banks[b0 + 1][:], lhsT=wg[:, c, :], rhs=xb_[:], start=True, stop=True),
                                 reads=["wg", N("xb")], writes=[BK(b0 + 1)])
                            S.op("pe", lambda e: e.matmul(banks[b0 + 2][:], lhsT=wg[:, 4 + c, :], rhs=xb_[:], start=True, stop=True),
                                 reads=["wg", N("xb")], writes=[BK(b0 + 2)])
                            yield
                            S.op("act", lambda e: e.activation(out=r_[:], in_=banks[b0 + 1][:], func=AF.Sigmoid, bias=lrup[:, 20 + c:21 + c]),
                                 reads=[BK(b0 + 1), "lrup"], writes=[N("r_")])
                            yield
                            S.op("act", lambda e: e.activation(out=i_[:], in_=banks[b0 + 2][:], func=AF.Sigmoid, bias=lrup[:, 24 + c:25 + c]),
                                 reads=[BK(b0 + 2), "lrup"], writes=[N("i_")])
                            yield
                            S.op("act", lambda e: e.activation(out=a_[:], in_=r_[:], func=AF.Exp, scale=lrup[:, 32 + c:33 + c]),
                                 reads=[N("r_"), "lrup"], writes=[N("a_")])
                            yield
                            S.op("act", lambda e: e.activation(out=u_[:], in_=r_[:], func=AF.Exp, scale=lrup[:, 36 + c:37 + c]),
                                 reads=[N("r_"), "lrup"], writes=[N("u_")])
                            yield
                            S.op("dve", lambda e: e.tensor_scalar(out=u_[:], in0=u_[:], scalar1=1.0, scalar2=None, op0=ALU.min),
                                 reads=[N("u_")], writes=[N("u_")])
                            yield
                            S.op("act", lambda e: e.activation(out=u_[:], in_=u_[:], func=AF.Sqrt, scale=-1.0, bias=1.0),
                                 reads=[N("u_")], writes=[N("u_")])
                            yield
                            S.op("dve", lambda e: e.tensor_tensor(out=i_[:], in0=i_[:], in1=xb_[:], op=ALU.mult),
                                 reads=[N("i_"), N("xb")], writes=[N("i_")])
                            yield
                            S.op("dve", lambda e: e.tensor_tensor(out=u_[:], in0=u_[:], in1=i_[:], op=ALU.mult),
                                 reads=[N("i_"), N("u_")], writes=[N("u_")])
                            yield
                            init = 0.0 if g == 0 else hh[1 - sl][:, 511:512]
                            S.op("dve", lambda e: e.tensor_tensor_scan(out=hh[sl][:], data0=a_[:], data1=u_[:], initial=init,
                                                                       op0=ALU.mult, op1=ALU.add),
                                 reads=[N("a_"), N("u_"), N(f"hh{1 - sl}")], writes=[N(f"hh{sl}")])
                            yield
                            proj(2048 + c * 128, g, b0 + 3)
                            yield
                            S.op("act", lambda e: e.activation(out=gg[:], in_=banks[b0 + 3][:], func=AF.Gelu_apprx_tanh),
                                 reads=[BK(b0 + 3)], writes=[N("gg")])
                            yield
                            S.op("dve", lambda e: e.tensor_tensor(out=rec[sl][:], in0=hh[sl][:], in1=gg[:], op=ALU.mult),
                                 reads=[N(f"hh{sl}"), N("gg")], writes=[N(f"rec{sl}")])
                            S.dma("sp", f"rst{k}{sl}", lambda e: e.dma_start(out=am_w[:, 4 * g:4 * g + 4, 4 + c, :],
                                                                             in_=rec[sl][:].rearrange("p (q j) -> p q j", q=4)),
                                  reads=[N(f"rec{sl}")], writes=[("rec_s", c)])
                            yield

                    for c0 in (0, 2):
                        ga, gb = lru_chain(c0, 0), lru_chain(c0 + 1, 1)
                        alive = [ga, gb]
                        while alive:
                            for gen in list(alive):
                                if next(gen, "done") == "done":
                                    alive.remove(gen)
                    S.barrier()

            stop_at("C")
          with ExitStack() as ph:
              woutb = T(ph, "woutb", [128, 8, D], BF16)
              wpqb = T(ph, "wpqb", [128, 8, D], BF16)
              skb = T(ph, "skb", [128, 8, 128], BF16)
              with ExitStack() as ph2:
                  wst = [T(ph2, f"wst2{i}", [128, D]) for i in range(2)]
                  skf = T(ph2, "skf", [128, 8 * 128])
                  n = 0
                  for (src, dstt, scaled) in ((w_out, woutb, True), (w_pq, wpqb, False)):
                      for kc in range(8):
                          sl = n % 2
                          n += 1
                          S.dma("sp", f"wst2{sl}", lambda e: e.dma_start(out=wst[sl][:], in_=src[kc * 128:(kc + 1) * 128, :]),
                                writes=[f"wst2{sl}"])
                          if scaled:
                              S.op("dve", lambda e: e.tensor_scalar(out=dstt[:, kc, :], in0=wst[sl][:], scalar1=gout[:, kc:kc + 1],
                                                                    scalar2=None, op0=ALU.mult),
                                   reads=[f"wst2{sl}", "gout"], writes=["wb"])
                          else:
                              S.op("pool", lambda e: e.tensor_copy(out=dstt[:, kc, :], in_=wst[sl][:]), reads=[f"wst2{sl}"], writes=["wb"])
                  S.dma("sp", "skf", lambda e: e.dma_start(out=skf[:], in_=skT_d), writes=["skf"])
                  S.op("dve", lambda e: e.tensor_copy(out=skb[:], in_=skf[:].rearrange("p (h n) -> p h n", h=8)), reads=["skf"], writes=["wb"])
                  S.barrier()

              xt = T(ph, "xt_e", [128, D])
              am = T(ph, "am", [128, 8, 128])
              amb = T(ph, "amb", [128, 8, 128], BF16)
              st = T(ph, "st_e", [128, 8])
              t1 = T(ph, "t1", [128, D])
              amq = t1[:].rearrange("p (k j) -> p k j", k=8)
              h2bs = [T(ph, f"h2b{i}", [128, D], BF16) for i in range(3)]
              junk = T(ph, "junk_e", [128, D], BF16)
              h2T = T(ph, "h2T", [128, 8, 128], BF16)
              qpT = T(ph, "qpT", [128, 8, 128], BF16)
              big8 = T(ph, "big8", [128, 2048])
              scr = big8[:].rearrange("p (h n) -> p h n", h=16)
              cand = big8[:].rearrange("p (h n) -> p h n", h=8)
              scws = [T(ph, f"scw{i}", [128, 128]) for i in range(2)]
              v12 = [T(ph, f"v12_{i}", [128, 8, 16]) for i in range(2)]
              i12 = [T(ph, f"i12_{i}", [128, 8, 16], U32) for i in range(2)]
              i12f = [T(ph, f"i12f_{i}", [128, 8, 16]) for i in range(2)]
              candws = [T(ph, f"candw{i}", [128, 256]) for i in range(2)]
              tv = T(ph, "tv", [128, 8, 16])
              tp = T(ph, "tp", [128, 8, 16], U32)
              ab = [T(ph, f"ab{i}", [128, 8, 16], U32) for i in range(2)]
              abf = [T(ph, f"abf{i}", [128, 8, 16]) for i in range(2)]
              isel = [T(ph, f"isel{i}", [128, 8, 16]) for i in range(2)]
              eidf = T(ph, "eidf", [128, 128])
              gs8 = T(ph, "gs8", [128, 8])
              x1s = [T(ph, f"x1_{i}", [128, D]) for i in range(3)]
              h2s = [T(ph, f"h2_{i}", [128, D]) for i in range(3)]
              eidxs = [T(ph, f"eidx{i}", [128, 128], I32) for i in range(3)]
              gsms = [T(ph, f"gsm{i}", [128, 8, 16]) for i in range(3)]
              NSL = 7
              uv = [T(ph, f"uv{i}", [128, GP, 2 * D], BF16) for i in range(NSL)]
              actp = T(ph, "actp", [128, 128])
              actg = T(ph, "actg", [128, 128])
              coef = T(ph, "coef", [128, 128])
              dg = [T(ph, f"dg{i}", [128, 128], BF16) for i in range(4)]
              junk2 = T(ph, "junk2", [128, D], BF16)
              junk3 = T(ph, "junk3", [128, D], BF16)
              NPR = 3
              prod = [T(ph, f"prod{i}", [128, D], BF16) for i in range(NPR)]
              ot = T(ph, "ot", [128, D])

              def X(tt):
                  par = tt % 3
                  x1, h2, eidx, gsm = x1s[par], h2s[par], eidxs[par], gsms[par]
                  h2b = h2bs[par]
                  RX1, RH2, REI, RGS = ("x1", par), ("h2", par), ("eidx", par), ("gsm", par)
                  RHB = ("h2b", par)
                  tok = slice(tt * 128, (tt + 1) * 128)
                  S.dma("sp", "xe", lambda e: e.dma_start(out=xt[:], in_=x[b, tok, :]), writes=["xt_e"])
                  S.dma("sp", "am_a", lambda e: e.dma_start(out=am[:], in_=am_s[tok, :].rearrange("p (k j) -> p k j", k=8)),
                        reads=["attn_s"] + [("rec_s", q) for q in range(4)], writes=["am_a", "am_r"])
                  yield
                  S.op("act", lambda e: e.activation(out=amb[:], in_=am[:], func=AF.Identity), reads=["am_a", "am_r"], writes=["amb"])
                  S.op("act", lambda e: e.activation(out=amq, in_=am[:], func=AF.Square), reads=["am_a", "am_r"], writes=["t1"])
                  yield
                  for kc in range(8):
                      hf = kc // 4
                      S.op("pe", lambda e: e.matmul(banks[3][:, hf * 2:hf * 2 + 2], lhsT=amq[:, kc, :], rhs=cst[:, 256:258],
                                                    start=(kc % 4 == 0), stop=(kc % 4 == 3)),
                           reads=["t1", "cst"], writes=[BK(3)])
                  yield
                  S.op("act", lambda e: e.activation(out=st[:, 0:4], in_=banks[3][:, 0:4], func=AF.Sqrt, scale=-1.0 / 512, bias=EPS),
                       reads=[BK(3)], writes=["st_e"])
                  S.op("dve", lambda e: e.reciprocal(out=st[:, 0:4], in_=st[:, 0:4]), reads=["st_e"], writes=["st_e"])
                  yield
                  for nh in range(2):
                      for part in range(2):
                          bk = nh * 2 + part
                          for k4 in range(4):
                              kc = part * 4 + k4
                              S.op("pe", lambda e: e.matmul(banks[bk][:], lhsT=amb[:, kc, :], rhs=woutb[:, kc, nh * 512:(nh + 1) * 512],
                                                            start=(k4 == 0), stop=(k4 == 3)),
                                   reads=["amb", "wb"], writes=[BK(bk)])
                          yield
                      cs = slice(nh * 512, (nh + 1) * 512)
                      S.op("act", lambda e: e.activation(out=t1[:, cs], in_=banks[nh * 2][:], func=AF.Copy, scale=st[:, 0:1]),
                           reads=[BK(nh * 2), "st_e"], writes=["t1"])
                      S.op("dve", lambda e: e.scalar_tensor_tensor(out=t1[:, cs], in0=banks[nh * 2 + 1][:], scalar=st[:, 2:3], in1=t1[:, cs],
                                                                   op0=ALU.mult, op1=ALU.add),
                           reads=[BK(nh * 2 + 1), "st_e", "t1"], writes=["t1"])
                      yield
                  S.op("dve", lambda e: e.tensor_tensor(out=t1[:], in0=t1[:], in1=mod[:, 0:1024], op=ALU.mult),
                       reads=["t1", "mod"], writes=["t1"])
                  S.op("dve", lambda e: e.tensor_tensor(out=x1[:], in0=t1[:], in1=xt[:], op=ALU.add),
                       reads=["t1", "xt_e"], writes=[RX1])
                  yield
                  if dbg:
                      S.dma("sp", "dbg", lambda e: e.dma_start(out=dbg_x1[tok, :], in_=x1[:]), reads=[RX1], writes=["dbg_x1"])
                  S.op("act", lambda e: e.activation(out=junk[:], in_=x1[:], func=AF.Square, accum_out=st[:, 4:5]),
                       reads=[RX1], writes=["junk_e", "st_e2"])
                  S.op("act", lambda e: e.activation(out=st[:, 5:6], in_=st[:, 4:5], func=AF.Sqrt, scale=1.0 / D, bias=EPS),
                       reads=["st_e2"], writes=["st_e2"])
                  S.op("dve", lambda e: e.reciprocal(out=st[:, 6:7], in_=st[:, 5:6]), reads=["st_e2"], writes=["st_e2"])
                  yield
                  S.op("dve", lambda e: e.scalar_tensor_tensor(out=h2[:], in0=x1[:], scalar=st[:, 6:7], in1=mod[:, 2048:3072],
                                                               op0=ALU.mult, op1=ALU.mult),
                       reads=[RX1, "st_e2", "mod"], writes=[RH2])
                  S.op("dve", lambda e: e.tensor_tensor(out=h2[:], in0=h2[:], in1=mod[:, 1024:2048], op=ALU.add),
                       reads=[RH2, "mod"], writes=[RH2])
                  S.op("act", lambda e: e.activation(out=h2b[:], in_=h2[:], func=AF.Identity), reads=[RH2], writes=[RHB])
                  yield
                  pv = banks[0][:].bitcast(BF16)
                  for kc in range(8):
                      S.op("pe", lambda e: e.transpose(out=pv[:, kc * 128:(kc + 1) * 128], in_=h2b[:, kc * 128:(kc + 1) * 128],
                                                       identity=identb[:]),
                           reads=[RHB, "identb"], writes=[BK(0)])
                  S.op("act", lambda e: e.activation(out=h2T[:], in_=pv.rearrange("p (k j) -> p k j", k=8), func=AF.Identity),
                       reads=[BK(0)], writes=["h2T"])
                  yield
                  for hd in range(8):
                      bk = 2 + hd // 4
                      for kc in range(8):
                          S.op("pe", lambda e: e.matmul(banks[bk][:, (hd % 4) * 128:(hd % 4 + 1) * 128], lhsT=wpqb[:, kc, hd * 128:(hd + 1) * 128],
                                                        rhs=h2T[:, kc, :], start=(kc == 0), stop=(kc == 7)),
                               reads=["wb", "h2T"], writes=[BK(bk)])
                      yield
                  for hf in range(2):
                      S.op("act", lambda e: e.activation(out=qpT[:, hf * 4:(hf + 1) * 4, :],
                                                         in_=banks[2 + hf][:].rearrange("p (h j) -> p h j", h=4), func=AF.Identity),
                           reads=[BK(2 + hf)], writes=["qpT"])
                  yield
                  for hd in range(8):
                      for half in range(2):
                          hh_ = half * 8 + hd
                          bk = hh_ // 4
                          pb = 64 * half
                          S.op("pe", lambda e: e.matmul(banks[bk][:, (hh_ % 4) * 128:(hh_ % 4 + 1) * 128], lhsT=qpT[pb:pb + 64, hd, :],
                                                        rhs=skb[pb:pb + 64, hd, :], start=True, stop=True),
                               reads=["qpT", "wb"], writes=[BK(bk)])
                  yield
                  for q in range(4):
                      S.op("act", lambda e: e.activation(out=scr[:, q * 4:(q + 1) * 4, :], in_=banks[q][:].rearrange("p (h j) -> p h j", h=4),
                                                         func=AF.Identity), reads=[BK(q)], writes=["big8"])
                  yield
                  for hd in range(8):
                      ch = [(v12[hf], i12[hf], scr[:, hf * 8 + hd, :], scws[hf], hf) for hf in range(2)]
                      for (vv, ii, src, sw, hf) in ch:
                          S.op("dve", lambda e: e.max(out=vv[:, hd, 0:8], in_=src), reads=["big8"], writes=[("v12", hf)])
                      for (vv, ii, src, sw, hf) in ch:
                          S.op("dve", lambda e: e.max_index(out=ii[:, hd, 0:8], in_max=vv[:, hd, 0:8], in_values=src),
                               reads=["big8", ("v12", hf)], writes=[("i12", hf)])
                      for (vv, ii, src, sw, hf) in ch:
                          S.op("dve", lambda e: e.match_replace(out=sw[:], in_to_replace=vv[:, hd, 0:8], in_values=src, imm_value=-1e30),
                               reads=["big8", ("v12", hf)], writes=[("scw", hf)])
                      yield
                      for (vv, ii, src, sw, hf) in ch:
                          S.op("dve", lambda e: e.max(out=vv[:, hd, 8:16], in_=sw[:]), reads=[("scw", hf)], writes=[("v12", hf)])
                      for (vv, ii, src, sw, hf) in ch:
                          S.op("dve", lambda e: e.max_index(out=ii[:, hd, 8:16], in_max=vv[:, hd, 8:16], in_values=sw[:]),
                               reads=[("scw", hf), ("v12", hf)], writes=[("i12", hf)])
                      yield
                  for half in range(2):
                      S.op("dve", lambda e: e.tensor_copy(out=i12f[half][:], in_=i12[half][:]), reads=[("i12", half)], writes=["i12f"])
                  c4 = cand.rearrange("p h (a b) -> p h a b", a=16)
                  S.op("dve", lambda e: e.tensor_tensor(out=c4, in0=v12[0][:].unsqueeze(3).to_broadcast([128, 8, 16, 16]),
                                                        in1=v12[1][:].unsqueeze(2).to_broadcast([128, 8, 16, 16]), op=ALU.add),
                       reads=[("v12", 0), ("v12", 1)], writes=["big8"])
                  yield
                  for h0 in range(0, 8, 2):
                      ch = [(h0 + q, cand[:, h0 + q, :], candws[q], q) for q in range(2)]
                      for (hd, src, cw, q) in ch:
                          S.op("dve", lambda e: e.max(out=tv[:, hd, 0:8], in_=src), reads=["big8"], writes=[("tv", q)])
                      for (hd, src, cw, q) in ch:
                          S.op("dve", lambda e: e.max_index(out=tp[:, hd, 0:8], in_max=tv[:, hd, 0:8], in_values=src),
                               reads=["big8", ("tv", q)], writes=[("tp", q)])
                      for (hd, src, cw, q) in ch:
                          S.op("dve", lambda e: e.match_replace(out=cw[:], in_to_replace=tv[:, hd, 0:8], in_values=src, imm_value=-1e30),
                               reads=["big8", ("tv", q)], writes=[("candw", q)])
                      yield
                      for (hd, src, cw, q) in ch:
                          S.op("dve", lambda e: e.max(out=tv[:, hd, 8:16], in_=cw[:]), reads=[("candw", q)], writes=[("tv", q)])
                      for (hd, src, cw, q) in ch:
                          S.op("dve", lambda e: e.max_index(out=tp[:, hd, 8:16], in_max=tv[:, hd, 8:16], in_values=cw[:]),
                               reads=[("candw", q), ("tv", q)], writes=[("tp", q)])
                      yield
                  S.op("dve", lambda e: e.tensor_single_scalar(out=ab[0][:], in_=tp[:], scalar=4, op=ALU.logical_shift_right), reads=[("tp", 0), ("tp", 1)], writes=["ab"])
                  S.op("dve", lambda e: e.tensor_single_scalar(out=ab[1][:], in_=tp[:], scalar=15, op=ALU.bitwise_and), reads=[("tp", 0), ("tp", 1)], writes=["ab"])
                  yield
                  e4 = cand.rearrange("p h (k a) -> p h k a", k=16)
                  io4 = iota_ka.rearrange("p (k a) -> p k a", k=16).unsqueeze(1).to_broadcast([128, 8, 16, 16])
                  for half in range(2):
                      S.op("dve", lambda e: e.tensor_copy(out=abf[half][:], in_=ab[half][:]), reads=["ab"], writes=["abf"])
                      S.op("dve", lambda e: e.tensor_tensor(out=e4, in0=abf[half][:].unsqueeze(3).to_broadcast([128, 8, 16, 16]), in1=io4, op=ALU.is_equal),
                           reads=["abf", "cst"], writes=["big8"])
                      yield
                      S.op("dve", lambda e: e.tensor_tensor(out=e4, in0=e4, in1=i12f[half][:].unsqueeze(2).to_broadcast([128, 8, 16, 16]), op=ALU.mult),
                           reads=["big8", "i12f"], writes=["big8"])
                      yield
                      S.op("dve", lambda e: e.reduce_sum(out=isel[half][:], in_=e4, axis=AX.X), reads=["big8"], writes=["isel"])
                      yield
                  S.op("dve", lambda e: e.scalar_tensor_tensor(out=eidf[:].rearrange("p (h k) -> p h k", h=8), in0=isel[0][:], scalar=128.0,
                                                               in1=isel[1][:], op0=ALU.mult, op1=ALU.add),
                       reads=["isel"], writes=["eidf"])
                  S.op("dve", lambda e: e.tensor_scalar(out=eidf[:], in0=eidf[:], scalar1=0.0, scalar2=16383.0, op0=ALU.max, op1=ALU.min),
                       reads=["eidf"], writes=["eidf"])
                  S.op("dve", lambda e: e.tensor_copy(out=eidx[:], in_=eidf[:]), reads=["eidf"], writes=[REI])
                  yield
                  S.op("dve", lambda e: e.tensor_tensor(out=gsm[:], in0=tv[:], in1=tv[:, :, 0:1].to_broadcast([128, 8, 16]), op=ALU.subtract),
                       reads=[("tv", 0), ("tv", 1)], writes=[RGS])
                  S.op("act", lambda e: e.activation(out=gsm[:], in_=gsm[:], func=AF.Exp), reads=[RGS], writes=[RGS])
                  S.op("dve", lambda e: e.reduce_sum(out=gs8[:], in_=gsm[:], axis=AX.X), reads=[RGS], writes=["gs8"])
                  S.op("dve", lambda e: e.reciprocal(out=gs8[:], in_=gs8[:]), reads=["gs8"], writes=["gs8"])
                  S.op("dve", lambda e: e.tensor_tensor(out=gsm[:], in0=gsm[:], in1=gs8[:].unsqueeze(2).to_broadcast([128, 8, 16]), op=ALU.mult),
                       reads=[RGS, "gs8"], writes=[RGS])
                  yield

              NGRP = 128 // GP
              slot_free = {}
              LAG = NSL - 2
              XYIELDS = 59

              NST = 3

              def issue(G):
                  tt, gi = divmod(G, NGRP)
                  par = tt % NST
                  eidx = eidxs[par]
                  sl = G % NSL
                  for p in range(GP):
                      j = gi * GP + p
                      S.dma("pool", f"g{sl}_{p}", lambda e: e.indirect_dma_start(out=uv[sl][:, p, :], out_offset=None, in_=uvb,
                                                                                 in_offset=bass.IndirectOffsetOnAxis(ap=eidx[:, j:j + 1], axis=0)),
                            reads=[("eidx", par), "uvb"], writes=[(f"uv{sl}", p)])

              def consume_a(G):
                  tt, gi = divmod(G, NGRP)
                  par = tt % NST
                  h2, h2b = h2s[par], h2bs[par]
                  sl = G % NSL
                  cs = slice(gi * GP, (gi + 1) * GP)
                  uvr = [(f"uv{sl}", q) for q in range(GP)]
                  for p in range(GP):
                      j = gi * GP + p
                      if p % 2 == 0:
                          S.op("dve", lambda e: e.scalar_tensor_tensor(out=junk2[:], in0=uv[sl][:, p, 0:D], scalar=1.0, in1=h2[:],
                                                                       op0=ALU.mult, op1=ALU.mult, accum_out=actp[:, j:j + 1]),
                               reads=uvr + [("h2", par)], writes=["junk2", ("actp", gi)])
                      else:
                          ps_ = G % NPR
                          S.op("dve", lambda e: e.tensor_tensor(out=prod[ps_][:], in0=uv[sl][:, p, 0:D], in1=h2b[:], op=ALU.mult),
                               reads=uvr + [("h2b", par)], writes=[f"prod{ps_}"])
                          S.op("act", lambda e: e.activation(out=junk3[:], in_=prod[ps_][:], func=AF.Identity, accum_out=actp[:, j:j + 1]),
                               reads=[f"prod{ps_}"], writes=["junk3", ("actp", gi)])
                  S.op("act", lambda e: e.activation(out=actg[:, cs], in_=actp[:, cs], func=AF.Gelu_apprx_tanh),
                       reads=[("actp", gi)], writes=[("actg", gi)])

              def consume_b(G):
                  tt, gi = divmod(G, NGRP)
                  par = tt % NST
                  gsm2 = gsms[par][:].rearrange("p h k -> p (h k)")
                  sl = G % NSL
                  cs = slice(gi * GP, (gi + 1) * GP)
                  uvr = [(f"uv{sl}", q) for q in range(GP)]
                  ab0 = 4 + 2 * (tt % 2)
                  S.op("dve", lambda e: e.tensor_tensor(out=coef[:, cs], in0=actg[:, cs], in1=gsm2[:, cs], op=ALU.mult),
                       reads=[("actg", gi), ("gsm", par)], writes=[("coef", gi)])
                  for p in range(GP):
                      j = gi * GP + p
                      ds = j % 4
                      S.op("act", lambda e: e.activation(out=dg[ds][:], in_=identb[:], func=AF.Copy, scale=coef[:, j:j + 1]),
                           reads=["identb", ("coef", gi)], writes=[f"dg{ds}"])
                      for nh in range(2):
                          S.op("pe", lambda e: e.matmul(banks[ab0 + nh][:], lhsT=dg[ds][:], rhs=uv[sl][:, p, D + nh * 512:D + (nh + 1) * 512],
                                                        start=(j == 0), stop=(j == 127)),
                               reads=[f"dg{ds}"] + uvr, writes=[BK(ab0 + nh)])
                  if gi == NGRP - 1:
                      x1 = x1s[par]
                      tok = slice(tt * 128, (tt + 1) * 128)
                      for nh in range(2):
                          cs2 = slice(nh * 512, (nh + 1) * 512)
                          S.op("dve", lambda e: e.tensor_tensor(out=ot[:, cs2], in0=banks[ab0 + nh][:],
                                                                in1=mod[:, 3072 + nh * 512:3072 + (nh + 1) * 512], op=ALU.mult),
                               reads=[BK(ab0 + nh), "mod"], writes=["ot"])
                      S.op("dve", lambda e: e.tensor_tensor(out=ot[:], in0=ot[:], in1=x1[:], op=ALU.add), reads=["ot", ("x1", par)], writes=["ot"])
                      S.dma("sp", "ost", lambda e: e.dma_start(out=out[b, tok, :], in_=ot[:]), reads=["ot"], writes=["out"])

              nyield = 0
              for _ in X(0):
                  nyield += 1
              assert nyield == XYIELDS, nyield
              NTOT = nt_de * NGRP
              xg = None
              xdone = 0
              XSPAN = NGRP - LAG - 3
              for G in range(NTOT + LAG + 1):
                  if G < NTOT:
                      issue(G)
                  if 0 <= G - LAG < NTOT:
                      consume_a(G - LAG)
                  if 0 <= G - LAG - 1 < NTOT:
                      consume_b(G - LAG - 1)
                  tcur, k = divmod(G, NGRP)
                  if k == 0 and tcur + 1 < nt_de:
                      xg = X(tcur + 1)
                      xdone = 0
                  if xg is not None:
                      tgt = min(XYIELDS, ((k + 1) * XYIELDS) // XSPAN + 1)
                      while xdone < tgt:
                          if next(xg, "done") == "done":
                              xg = None
                              break
                          xdone += 1
              S.barrier()
        S.barrier(["sp"])
    return nc


def _consts():
    c = np.zeros((128, NCONST), np.float32)
    c[:, 0:128] = np.eye(128, dtype=np.float32)
    jj = np.arange(128)[:, None]
    ss = np.arange(128)[None, :]
    c[:, 128:256] = -(jj >= ss).astype(np.float32)
    c[:, 256:384] = -1.0
    c[:, 384:512] = ((jj // 64) == (ss // 64)).astype(np.float32)
    c[:, 512:768] = np.tile(np.arange(16, dtype=np.float32), 16)[None, :]
    return c


def _masks():
    m = np.zeros((128, 2048), np.float32)
    jj = np.arange(128)[:, None]
    t = np.arange(512)[None, :]
    for j in range(4):
        m[:, j * 512:(j + 1) * 512] = ((128 * j + jj) < t).astype(np.float32)
    return m


def _prep(inputs):
    f = lambda k: np.ascontiguousarray(np.asarray(inputs[k], dtype=np.float32))
    x = f("x"); c = f("c")
    shared = {
        "w_mod": f("w_mod")[0], "b_mod": f("b_mod"), "g1": f("g_norm1"), "g2": f("g_norm2"),
        "w_in": f("w_in")[0], "w_out": f("w_out")[0], "w_pq": f("w_pq")[0],
        "expert_u": f("expert_u")[0], "expert_v": f("expert_v")[0], "consts": _consts(), "masks": _masks(),
    }
    gq = f("g_q")[0]; gk = f("g_k")[0]
    shared["gqk"] = np.ascontiguousarray(np.stack([np.tile(gq, 2), np.tile(gk, 2)], axis=1))
    lr = np.zeros((128, 32), np.float32)
    cw = f("conv_w")[0]
    for cch in range(4):
        for k in range(4):
            lr[:, cch * 4 + k] = cw[k, cch * 128:(cch + 1) * 128]
    lr[:, 16:20] = f("conv_b")[0].reshape(4, 128).T
    lr[:, 20:24] = f("b_rg")[0].reshape(4, 128).T
    lr[:, 24:28] = f("b_ig")[0].reshape(4, 128).T
    lr[:, 28:32] = f("lru_lambda")[0].reshape(4, 128).T
    shared["lrup"] = lr
    wg = np.zeros((128, 8, 128), np.float32)
    for gi, key in enumerate(("w_rg", "w_ig")):
        w = f(key)[0]
        for cch in range(4):
            wg[0:64, gi * 4 + cch, 0:64] = w[2 * cch]
            wg[64:128, gi * 4 + cch, 64:128] = w[2 * cch + 1]
    shared["wg_bd"] = wg.reshape(128, 1024)
    go = np.concatenate([f("g_out_attn")[0], f("g_out_lru")[0]])
    shared["gout"] = np.ascontiguousarray(go.reshape(8, 128).T)
    sk1 = f("sub_keys1")[0]; sk2 = f("sub_keys2")[0]
    sk = np.concatenate([sk1.transpose(2, 0, 1), sk2.transpose(2, 0, 1)], axis=0)
    shared["skT"] = np.ascontiguousarray(sk.reshape(128, 1024))
    maps = []
    for i in range(NCORES):
        m = dict(shared)
        m["x"] = np.ascontiguousarray(x[2 * i:2 * i + 2])
        cc = c[2 * i:2 * i + 2]
        m["cT"] = np.ascontiguousarray(cc.reshape(2, 8, 128).transpose(2, 1, 0).reshape(128, 16))
        maps.append(m)
    return maps


def kernel(**inputs):
    maps = _prep(inputs)
    nc = build_nc()
    res = run_bass_kernel_spmd(nc, maps, core_ids=list(range(NCORES)))
    outs = [np.asarray(r["out"]) for r in res.results]
    return np.concatenate(outs, axis=0).astype(np.float32)
```

```python
import numpy as np
from contextlib import ExitStack
import concourse.bass as bass
import concourse.mybir as mybir
from concourse.bass_utils import run_bass_kernel_spmd

F32 = mybir.dt.float32
BF16 = mybir.dt.bfloat16
U32 = mybir.dt.uint32
I32 = mybir.dt.int32
AF = mybir.ActivationFunctionType
ALU = mybir.AluOpType
AX = mybir.AxisListType

NCORES = 8
SEQ = 4096
D = 1024
EPS = 1e-6
NT = SEQ // 128
NG = SEQ // 512
GP = 2
NCONST = 128 * 4 + 256


class Sched:
    def __init__(self, nc, es):
        self.nc = nc
        self.engs = {"pe": nc.tensor, "act": nc.scalar, "dve": nc.vector, "pool": nc.gpsimd, "sp": nc.sync}
        self.sem = {k: es.enter_context(nc.semaphore("s_" + k)) for k in self.engs}
        self.cnt = {k: 0 for k in self.engs}
        self.seen = {k: {} for k in self.engs}
        self.res = {}
        self.dsem = {}
        self.dsem_by_id = {}
        self.es = es

    def _need(self, reads, writes):
        need = []
        for r in reads:
            st = self.res.get(r)
            if st and st["w"]:
                need.append(st["w"])
        for w in writes:
            st = self.res.get(w)
            if st:
                if st["w"]:
                    need.append(st["w"])
                need.extend(st["r"].values())
        return need

    def _wait(self, eng, need):
        best = {}
        for (s, v, src) in need:
            if src == "pe" and eng == "pe":
                continue
            k = id(s)
            if k not in best or best[k][1] < v:
                best[k] = (s, v)
        for k, (s, v) in best.items():
            if k in self.dsem_by_id:
                v = self.dsem_by_id[k][1]
            if self.seen[eng].get(k, -1) >= v:
                continue
            self.engs[eng].wait_ge(s, v)
            self.seen[eng][k] = v

    def _upd(self, tok, reads, writes):
        for r in reads:
            st = self.res.setdefault(r, {"w": None, "r": {}})
            k = id(tok[0])
            if k not in st["r"] or st["r"][k][1] < tok[1]:
                st["r"][k] = tok
        for w in writes:
            self.res[w] = {"w": tok, "r": {}}

    def op(self, eng, fn, reads=(), writes=()):
        self._wait(eng, self._need(reads, writes))
        ins = fn(self.engs[eng])
        self.cnt[eng] += 1
        ins.then_inc(self.sem[eng], 1)
        self._upd((self.sem[eng], self.cnt[eng], eng), reads, writes)
        return ins

    def dma(self, eng, group, fn, reads=(), writes=(), after=None):
        if group not in self.dsem:
            self.dsem[group] = [self.es.enter_context(self.nc.semaphore("d_" + str(group))), 0]
            self.dsem_by_id[id(self.dsem[group][0])] = self.dsem[group]
        if after is None:
            self._wait(eng, self._need(reads, writes))
        else:
            self._wait(eng, self._need(reads, ()) + list(after))
        ins = fn(self.engs[eng])
        d = self.dsem[group]
        d[1] += 16
        ins.then_inc(d[0], 16)
        self._upd((d[0], d[1], "dma"), reads, writes)
        return ins

    def barrier(self, engs=None):
        allv = [(self.sem[k], self.cnt[k], k + "_b") for k in self.engs if self.cnt[k] > 0]
        allv += [(d[0], d[1], "dma") for d in self.dsem.values() if d[1] > 0]
        for e in (engs or self.engs):
            self._wait(e, allv)


class _Stop(Exception):
    pass


def build_nc(nseq=2, nt_de=NT, do_peer=True, dbg=False, stop=None):
    try:
        return _build_nc(nseq, nt_de, do_peer, dbg, stop)
    except _Stop as e:
        return e.args[0]


def _build_nc(nseq, nt_de, do_peer, dbg, stop):
    nc = bass.Bass("TRN2", target_bir_lowering=False)
    dram = lambda n, s, dt=F32, kind="ExternalInput": nc.dram_tensor(n, list(s), dt, kind=kind).ap()
    x = dram("x", [2, SEQ, D])
    cT = dram("cT", [128, 16])
    w_mod = dram("w_mod", [D, 6 * D])
    b_mod = dram("b_mod", [1, 6 * D])
    g1 = dram("g1", [1, D])
    g2 = dram("g2", [1, D])
    w_in = dram("w_in", [D, 2560])
    gqk_d = dram("gqk", [128, 2])
    lrup_d = dram("lrup", [128, 32])
    wg_d = dram("wg_bd", [128, 8 * 128])
    gout_d = dram("gout", [128, 8])
    w_out = dram("w_out", [D, D])
    w_pq = dram("w_pq", [D, D])
    skT_d = dram("skT", [128, 8 * 128])
    eu = dram("expert_u", [16384, D])
    ev = dram("expert_v", [16384, D])
    consts_d = dram("consts", [128, NCONST])
    masks_d = dram("masks", [128, 2048])
    out = dram("out", [2, SEQ, D], kind="ExternalOutput")
    am_s = dram("am_s", [SEQ, D], kind="ExternalOutput" if dbg else "Internal")
    am_w = am_s.rearrange("(t p) (k j) -> p t k j", p=128, k=8)
    uvb = dram("uvb", [16384, 2 * D], BF16, kind="Internal")
    if dbg:
        dbg_x1 = dram("dbg_x1", [SEQ, D], kind="ExternalOutput")
        dbg_i = dram("dbg_i", [128, 128], I32, kind="ExternalOutput")
        dbg_g = dram("dbg_g", [128, 128], kind="ExternalOutput")
        dbg_a = dram("dbg_a", [128, 128], kind="ExternalOutput")

    with ExitStack() as es:
        S = Sched(nc, es)
        banks = [es.enter_context(nc.psum_tensor(f"bank{i}", [128, 512], F32)) for i in range(8)]
        BK = lambda i: ("bank", i)

        uid = [0]

        def T(stack, name, shape, dt=F32):
            uid[0] += 1
            return stack.enter_context(nc.sbuf_tensor(f"{name}_{uid[0]}", list(shape), dt))

        def stop_at(tag):
            if stop == tag:
                S.barrier()
                raise _Stop(nc)

        cst = T(es, "cst", [128, NCONST])
        identb = T(es, "identb", [128, 128], BF16)
        mnegb = T(es, "mnegb", [128, 128], BF16)
        negonesb = T(es, "negonesb", [128, 128], BF16)
        maskb = T(es, "maskb", [128, 4, 512], BF16)
        sc = T(es, "sc", [128, 8, 2])
        gqk = T(es, "gqk_t", [128, 2])
        lrup = T(es, "lrup_t", [128, 40])
        wg = T(es, "wg_t", [128, 8, 128])
        gout = T(es, "gout_t", [128, 8])
        mod = T(es, "mod", [128, 4 * D])
        ident_f = cst[:, 0:128]
        negones_f = cst[:, 256:384]
        blockones_f = cst[:, 384:512]
        iota_ka = cst[:, 512:768]

        S.dma("sp", "c0", lambda e: e.dma_start(out=cst[:], in_=consts_d), writes=["cst"])
        S.dma("sp", "c0", lambda e: e.dma_start(out=sc[:], in_=cT.rearrange("p (k b) -> p k b", b=2)), writes=["sc"])
        S.dma("sp", "c0", lambda e: e.dma_start(out=gqk[:], in_=gqk_d), writes=["gqk"])
        S.dma("sp", "c0", lambda e: e.dma_start(out=lrup[:, 0:32], in_=lrup_d), writes=["lrup"])
        S.dma("sp", "c0", lambda e: e.dma_start(out=wg[:], in_=wg_d.rearrange("p (c j) -> p c j", j=128)), writes=["wg"])
        S.dma("sp", "c0", lambda e: e.dma_start(out=gout[:], in_=gout_d), writes=["gout"])
        with ExitStack() as ph:
            mst = T(ph, "mst", [128, 2048])
            S.dma("sp", "c0", lambda e: e.dma_start(out=mst[:], in_=masks_d), writes=["mst"])
            S.barrier()
            S.op("dve", lambda e: e.tensor_copy(out=maskb[:], in_=mst[:].rearrange("p (j t) -> p j t", j=4)),
                 reads=["mst"], writes=["maskb"])
            S.barrier()
        S.op("dve", lambda e: e.tensor_copy(out=identb[:], in_=cst[:, 0:128]), reads=["cst"], writes=["identb"])
        S.op("dve", lambda e: e.tensor_copy(out=mnegb[:], in_=cst[:, 128:256]), reads=["cst"], writes=["mnegb"])
        S.op("dve", lambda e: e.tensor_copy(out=negonesb[:], in_=cst[:, 256:384]), reads=["cst"], writes=["negonesb"])
        S.op("act", lambda e: e.activation(out=sc[:], in_=sc[:], func=AF.Silu), reads=["sc"], writes=["sc"])
        S.op("dve", lambda e: e.tensor_scalar(out=gqk[:, 0:1], in0=gqk[:, 0:1], scalar1=0.125, scalar2=None, op0=ALU.mult),
             reads=["gqk"], writes=["gqk"])
        S.op("act", lambda e: e.activation(out=lrup[:, 32:36], in_=lrup[:, 28:32], func=AF.Exp, scale=-1.0), reads=["lrup"], writes=["lrup"])
        S.op("act", lambda e: e.activation(out=lrup[:, 32:36], in_=lrup[:, 32:36], func=AF.Ln, bias=1.0), reads=["lrup"], writes=["lrup"])
        S.op("dve", lambda e: e.tensor_scalar(out=lrup[:, 36:40], in0=lrup[:, 32:36], scalar1=-16.0, scalar2=None, op0=ALU.mult),
             reads=["lrup"], writes=["lrup"])
        S.op("dve", lambda e: e.tensor_scalar(out=lrup[:, 32:36], in0=lrup[:, 32:36], scalar1=-8.0, scalar2=None, op0=ALU.mult),
             reads=["lrup"], writes=["lrup"])

        for b in range(nseq):
          with ExitStack() as sq:
            modA = T(sq, "modA", [128, 2 * D])
            with ExitStack() as ph:
                screp = T(ph, "screp", [128, 8, 128])
                wm = [T(ph, f"wm{i}", [128, 8, 512]) for i in range(2)]
                gbc = T(ph, "gbc", [128, D])
                onesr = T(ph, "onesr", [1, 128])
                bmod = [T(ph, f"bmod{i}", [1, 512]) for i in range(2)]
                S.op("dve", lambda e: e.memset(onesr[:], 1.0), writes=["onesr"])
                S.op("dve", lambda e: e.tensor_copy(out=screp[:], in_=sc[:, :, b:b + 1].to_broadcast([128, 8, 128])),
                     reads=["sc"], writes=["screp"])
                wmv = w_mod.rearrange("(kc p) n -> p kc n", p=128)
                for nb in range(12):
                    sl = nb % 2
                    S.dma("sp", f"bm{sl}", lambda e: e.dma_start(out=bmod[sl][:], in_=b_mod[0:1, nb * 512:(nb + 1) * 512]),
                          writes=[f"bmod{sl}"])
                    S.dma("sp", f"wm{sl}", lambda e: e.dma_start(out=wm[sl][:], in_=wmv[:, :, nb * 512:(nb + 1) * 512]),
                          writes=[f"wm{sl}"])
                    bk = nb % 2
                    for kc in range(8):
                        S.op("pe", lambda e: e.matmul(banks[bk][:], lhsT=screp[:, kc, :], rhs=wm[sl][:, kc, :],
                                                      start=(kc == 0), stop=False),
                             reads=["screp", f"wm{sl}"], writes=[BK(bk)])
                    S.op("pe", lambda e: e.matmul(banks[bk][:], lhsT=onesr[:], rhs=bmod[sl][:],
                                                  start=False, stop=True),
                         reads=["onesr", f"bmod{sl}"], writes=[BK(bk)])
                    mdst = modA[:, nb * 512:(nb + 1) * 512] if nb < 4 else mod[:, (nb - 4) * 512:(nb - 3) * 512]
                    S.op("act", lambda e: e.activation(out=mdst, in_=banks[bk][:], func=AF.Identity),
                         reads=[BK(bk)], writes=["mod"])
                for (gd, mt, c0) in ((g1, modA, 1024), (g2, mod, 2048)):
                    S.dma("sp", "gbc", lambda e: e.dma_start(out=gbc[:], in_=gd.to_broadcast([128, D])), writes=["gbc"])
                    S.op("dve", lambda e: e.scalar_tensor_tensor(out=mt[:, c0:c0 + D], in0=mt[:, c0:c0 + D], scalar=1.0,
                                                                 in1=gbc[:], op0=ALU.add, op1=ALU.mult),
                         reads=["gbc", "mod"], writes=["mod"])
                S.barrier()

            stop_at("M")
            with ExitStack() as ph:
                winb = T(ph, "winb", [128, 8, 2560], BF16)
                hT = T(ph, "hT", [128, 8, SEQ], BF16)
                with ExitStack() as ph2:
                    wst = [T(ph2, f"wst{i}", [128, 2560]) for i in range(2)]
                    for kc in range(8):
                        sl = kc % 2
                        S.dma("sp", f"wst{sl}", lambda e: e.dma_start(out=wst[sl][:], in_=w_in[kc * 128:(kc + 1) * 128, :]),
                              writes=[f"wst{sl}"])
                        eng = "dve" if kc % 2 == 0 else "pool"
                        S.op(eng, lambda e: e.tensor_copy(out=winb[:, kc, :], in_=wst[sl][:]), reads=[f"wst{sl}"], writes=["winb"])
                    S.barrier()
                with ExitStack() as ph2:
                    xt = [T(ph2, f"xt{i}", [128, D]) for i in range(2)]
                    hn = [T(ph2, f"hn{i}", [128, D]) for i in range(2)]
                    hb = [T(ph2, f"hb{i}", [128, D], BF16) for i in range(2)]
                    junk = T(ph2, "junk", [128, D], BF16)
                    ss = [T(ph2, f"ss{i}", [128, 4]) for i in range(2)]
                    def a1(tt):
                        sl = tt % 2
                        S.dma("sp", f"x{sl}", lambda e: e.dma_start(out=xt[sl][:], in_=x[b, tt * 128:(tt + 1) * 128, :]),
                              writes=[f"xt{sl}"])
                        S.op("act", lambda e: e.activation(out=junk[:], in_=xt[sl][:], func=AF.Square, accum_out=ss[sl][:, 0:1]),
                             reads=[f"xt{sl}"], writes=["junk", f"ss{sl}"])
                        S.op("act", lambda e: e.activation(out=ss[sl][:, 1:2], in_=ss[sl][:, 0:1], func=AF.Sqrt, scale=1.0 / D, bias=EPS),
                             reads=[f"ss{sl}"], writes=[f"ss{sl}"])
                        S.op("dve", lambda e: e.reciprocal(out=ss[sl][:, 2:3], in_=ss[sl][:, 1:2]), reads=[f"ss{sl}"], writes=[f"ss{sl}"])
                        S.op("dve", lambda e: e.scalar_tensor_tensor(out=hn[sl][:], in0=xt[sl][:], scalar=ss[sl][:, 2:3],
                                                                     in1=modA[:, 1024:2048], op0=ALU.mult, op1=ALU.mult),
                             reads=[f"xt{sl}", f"ss{sl}", "mod"], writes=[f"hn{sl}"])
                        S.op("pool", lambda e: e.tensor_tensor(out=hb[sl][:], in0=hn[sl][:], in1=modA[:, 0:1024], op=ALU.add),
                             reads=[f"hn{sl}", "mod"], writes=[f"hb{sl}"])

                    def a2(tt):
                        sl = tt % 2
                        bk = 6 + sl
                        pv = banks[bk][:].bitcast(BF16)
                        for kc in range(8):
                            S.op("pe", lambda e: e.transpose(out=pv[:, kc * 128:(kc + 1) * 128], in_=hb[sl][:, kc * 128:(kc + 1) * 128],
                                                             identity=identb[:]),
                                 reads=[f"hb{sl}", "identb"], writes=[BK(bk)])
                        S.op("act", lambda e: e.activation(out=hT[:, :, tt * 128:(tt + 1) * 128],
                                                           in_=pv.rearrange("p (k j) -> p k j", k=8), func=AF.Identity),
                             reads=[BK(bk)], writes=[("hT", tt // 4)])

                    for tt in range(NT + 1):
                        if tt < NT:
                            a1(tt)
                        if tt >= 1:
                            a2(tt - 1)
                    S.barrier()

                stop_at("A")

                def proj(col0, g, bk):
                    for kc in range(8):
                        S.op("pe", lambda e: e.matmul(banks[bk][:], lhsT=winb[:, kc, col0:col0 + 128],
                                                      rhs=hT[:, kc, g * 512:(g + 1) * 512], start=(kc == 0), stop=(kc == 7)),
                             reads=["winb", ("hT", g)], writes=[BK(bk)])

                with ExitStack() as ph2:
                    kT = T(ph2, "kT", [128, SEQ], BF16)
                    qT = T(ph2, "qT", [128, SEQ], BF16)
                    V = T(ph2, "V", [128, NT, 128], BF16)
                    sq = T(ph2, "sq", [128, 512])
                    rs = T(ph2, "rs", [128, 512])
                    NSB = 3
                    E = [T(ph2, f"E{i}", [128, 512]) for i in range(NSB)]
                    L = [T(ph2, f"L{i}", [128, 512], BF16) for i in range(NSB)]
                    W = [T(ph2, f"W{i}", [128, 512], BF16) for i in range(NSB)]
                    cum = [T(ph2, f"cum{i}", [128, 512], BF16) for i in range(2)]
                    osb = [T(ph2, f"osb{i}", [64, 512]) for i in range(2)]
                    pg = None
                    if b == 0:
                        cin = [T(ph2, f"cin{i}", [128, D]) for i in range(2)]
                        cout = [T(ph2, f"cout{i}", [128, D], BF16) for i in range(2)]

                        def prepass():
                            n = 0
                            for ti, src in enumerate((eu, ev)):
                                for ch in range(128):
                                    sl = n % 2
                                    n += 1
                                    S.dma("sp", f"cin{sl}", lambda e: e.dma_start(out=cin[sl][:], in_=src[ch * 128:(ch + 1) * 128, :]),
                                          writes=[f"cin{sl}"])
                                    S.op("pool", lambda e: e.tensor_copy(out=cout[sl][:], in_=cin[sl][:]), reads=[f"cin{sl}"], writes=[f"cout{sl}"])
                                    S.dma("pool", f"cst{sl}", lambda e: e.dma_start(out=uvb[ch * 128:(ch + 1) * 128, ti * D:(ti + 1) * D], in_=cout[sl][:]),
                                          reads=[f"cout{sl}"], writes=["uvb"])
                                    yield
                        pg = prepass()
                    for hp in range(4):
                        for (dst, dname, col0, gcol) in ((kT, "kT", 512 + hp * 128, 1), (qT, "qT", hp * 128, 0)):
                            for g in range(NG):
                                bk = 6 + (g % 2)
                                proj(col0, g, bk)
                                S.op("act", lambda e: e.activation(out=sq[:], in_=banks[bk][:], func=AF.Square),
                                     reads=[BK(bk)], writes=["sq"])
                                S.op("pe", lambda e: e.matmul(banks[4][:], lhsT=blockones_f, rhs=sq[:], start=True, stop=True),
                                     reads=["sq", "cst"], writes=[BK(4)])
                                S.op("act", lambda e: e.activation(out=rs[:], in_=banks[4][:], func=AF.Sqrt, scale=1.0 / 64, bias=EPS),
                                     reads=[BK(4)], writes=["rs"])
                                S.op("dve", lambda e: e.reciprocal(out=rs[:], in_=rs[:]), reads=["rs"], writes=["rs"])
                                S.op("dve", lambda e: e.scalar_tensor_tensor(out=dst[:, g * 512:(g + 1) * 512], in0=banks[bk][:],
                                                                             scalar=gqk[:, gcol:gcol + 1], in1=rs[:],
                                                                             op0=ALU.mult, op1=ALU.mult),
                                     reads=[BK(bk), "gqk", "rs"], writes=[(dname, g)])
                        for tb4 in range(NT // 4):
                            bk = 6 + (tb4 % 2)
                            for q4 in range(4):
                                tb = tb4 * 4 + q4
                                for kc in range(8):
                                    S.op("pe", lambda e: e.matmul(banks[bk][:, q4 * 128:(q4 + 1) * 128],
                                                                  lhsT=hT[:, kc, tb * 128:(tb + 1) * 128],
                                                                  rhs=winb[:, kc, 1024 + hp * 128:1024 + (hp + 1) * 128],
                                                                  start=(kc == 0), stop=(kc == 7)),
                                         reads=["winb", ("hT", tb // 4)], writes=[BK(bk)])
                            S.op("dve", lambda e: e.tensor_copy(out=V[:, tb4 * 4:(tb4 + 1) * 4, :],
                                                                in_=banks[bk][:].rearrange("p (q j) -> p q j", q=4)),
                                 reads=[BK(bk)], writes=[("V", tb4)])
                        pairs = []
                        for h2 in range(2):
                            for g in range(NG):
                                for idx, kb in enumerate(range(4 * g + 3, -1, -1)):
                                    pairs.append((h2, g, kb, idx, idx == 0, kb == 0, kb - 4 * g, h2 * NG + g))

                        def s1(n):
                            h2, g, kb, idx, first, last, j, run = pairs[n]
                            sl = n % NSB
                            ba = n % 4
                            pb = 64 * h2
                            S.op("pe", lambda e: e.matmul(banks[ba][:], lhsT=kT[pb:pb + 64, kb * 128:(kb + 1) * 128],
                                                          rhs=qT[pb:pb + 64, g * 512:(g + 1) * 512], start=True, stop=True),
                                 reads=[("kT", kb // 4), ("qT", g)], writes=[BK(ba)])
                            S.op("act", lambda e: e.activation(out=E[sl][:], in_=banks[ba][:], func=AF.Exp),
                                 reads=[BK(ba)], writes=[f"E{sl}"])
                            S.op("act", lambda e: e.activation(out=L[sl][:], in_=E[sl][:], func=AF.Ln, bias=1.0),
                                 reads=[f"E{sl}"], writes=[f"L{sl}"])
                            if j >= 0:
                                S.op("dve", lambda e: e.tensor_tensor(out=L[sl][:], in0=L[sl][:], in1=maskb[:, j, :], op=ALU.mult),
                                     reads=[f"L{sl}", "maskb"], writes=[f"L{sl}"])

                        def s2(n):
                            h2, g, kb, idx, first, last, j, run = pairs[n]
                            sl = n % NSB
                            bk = n % 4
                            pb = 64 * h2
                            S.op("pe", lambda e: e.matmul(banks[bk][:], lhsT=mnegb[:], rhs=L[sl][:], start=False, stop=first),
                                 reads=["mnegb", f"L{sl}"], writes=[BK(bk)])
                            if not first:
                                S.op("pe", lambda e: e.matmul(banks[bk][:], lhsT=negonesb[:], rhs=cum[idx % 2][:], start=False, stop=True),
                                     reads=["negonesb", f"cum{idx % 2}"], writes=[BK(bk)])
                            S.op("act", lambda e: e.activation(out=W[sl][:], in_=banks[bk][:], func=AF.Exp),
                                 reads=[BK(bk)], writes=[f"W{sl}"])
                            if j >= 0:
                                S.op("dve", lambda e: e.tensor_tensor(out=W[sl][:], in0=W[sl][:], in1=maskb[:, j, :], op=ALU.mult),
                                     reads=[f"W{sl}", "maskb"], writes=[f"W{sl}"])
                            if not last:
                                nx = (idx + 1) % 2
                                if first:
                                    S.op("dve", lambda e: e.tensor_copy(out=cum[nx][:], in_=L[sl][:]),
                                         reads=[f"L{sl}"], writes=[f"cum{nx}"])
                                else:
                                    S.op("dve", lambda e: e.tensor_tensor(out=cum[nx][:], in0=cum[idx % 2][:], in1=L[sl][:], op=ALU.add),
                                         reads=[f"L{sl}", f"cum{idx % 2}"], writes=[f"cum{nx}"])

                        def s3(n):
                            h2, g, kb, idx, first, last, j, run = pairs[n]
                            sl = n % NSB
                            ob = 4 + (run % 2)
                            S.op("pe", lambda e: e.matmul(banks[ob][0:64, :], lhsT=V[:, kb, h2 * 64:(h2 + 1) * 64], rhs=W[sl][:],
                                                          start=first, stop=last),
                                 reads=[("V", kb // 4), f"W{sl}"], writes=[BK(ob)])
                            if last:
                                o = run % 2
                                S.op("dve", lambda e: e.tensor_copy(out=osb[o][:], in_=banks[ob][0:64, :]),
                                     reads=[BK(ob)], writes=[f"osb{o}"])
                                r0 = (hp * 2 + h2) * 64
                                S.dma("sp", f"ost{o}", lambda e: e.dma_start(out=am_w[64 * h2:64 * h2 + 64, 4 * g:4 * g + 4, hp, :],
                                                                             in_=osb[o][:].rearrange("p (q j) -> p q j", q=4)),
                                      reads=[f"osb{o}"], writes=["attn_s"])

                        NP = len(pairs)
                        for m in range(NP + 2):
                            if pg is not None and m % 4 == 0:
                                next(pg, None)
                            if m < NP:
                                s1(m)
                            if 0 <= m - 1 < NP:
                                s2(m - 1)
                            if 0 <= m - 2 < NP:
                                s3(m - 2)
                    if pg is not None:
                        for _ in pg:
                            pass
                    S.barrier()

                stop_at("B")
                with ExitStack() as ph2:
                    def mk(k):
                        d = dict(lx=[T(ph2, f"lx{k}{i}", [128, 3 + 512]) for i in range(2)],
                                 hh=[T(ph2, f"hh{k}{i}", [128, 512]) for i in range(2)],
                                 rec=[T(ph2, f"rec{k}{i}", [128, 512]) for i in range(2)])
                        for nm in ("xb", "r_", "i_", "a_", "u_", "gg"):
                            d[nm] = T(ph2, f"{nm}{k}", [128, 512])
                        return d
                    cb = [mk(0), mk(1)]

                    def lru_chain(c, k):
                        B_ = cb[k]
                        lx, hh, rec = B_["lx"], B_["hh"], B_["rec"]
                        xb_, r_, i_, a_, u_, gg = B_["xb"], B_["r_"], B_["i_"], B_["a_"], B_["u_"], B_["gg"]
                        b0 = 4 * k
                        N = lambda nm: f"{nm}{k}"
                        for g in range(NG):
                            sl = g % 2
                            proj(1536 + c * 128, g, b0)
                            yield
                            if g == 0:
                                S.op("dve", lambda e: e.memset(lx[sl][:, 0:3], 0.0), writes=[N(f"lx{sl}")])
                            S.op("act", lambda e: e.activation(out=lx[sl][:, 3:515], in_=banks[b0][:], func=AF.Identity),
                                 reads=[BK(b0)], writes=[N(f"lx{sl}")])
                            yield
                            if g + 1 < NG:
                                S.op("dve", lambda e: e.tensor_copy(out=lx[1 - sl][:, 0:3], in_=lx[sl][:, 512:515]),
                                     reads=[N(f"lx{sl}")], writes=[N(f"lx{1 - sl}")])
                            S.op("dve", lambda e: e.tensor_scalar(out=xb_[:], in0=lx[sl][:, 0:512], scalar1=lrup[:, c * 4:c * 4 + 1],
                                                                  scalar2=lrup[:, 16 + c:17 + c], op0=ALU.mult, op1=ALU.add),
                                 reads=[N(f"lx{sl}"), "lrup"], writes=[N("xb")])
                            yield
                            for kk in range(1, 4):
                                S.op("dve", lambda e: e.scalar_tensor_tensor(out=xb_[:], in0=lx[sl][:, kk:kk + 512],
                                                                             scalar=lrup[:, c * 4 + kk:c * 4 + kk + 1], in1=xb_[:],
                                                                             op0=ALU.mult, op1=ALU.add),
                                     reads=[N(f"lx{sl}"), "lrup", N("xb")], writes=[N("xb")])
                                yield
                            S.op("pe", lambda e: e.matmul(banks[b0 + 1][:], lhsT=wg[:, c, :], rhs=xb_[:], start=True, stop=True),
                                 reads=["wg", N("xb")], writes=[BK(b0 + 1)])
                            S.op("pe", lambda e: e.matmul(banks[b0 + 2][:], lhsT=wg[:, 4 + c, :], rhs=xb_[:], start=True, stop=True),
                                 reads=["wg", N("xb")], writes=[BK(b0 + 2)])
                            yield
                            S.op("act", lambda e: e.activation(out=r_[:], in_=banks[b0 + 1][:], func=AF.Sigmoid, bias=lrup[:, 20 + c:21 + c]),
                                 reads=[BK(b0 + 1), "lrup"], writes=[N("r_")])
                            yield
                            S.op("act", lambda e: e.activation(out=i_[:], in_=banks[b0 + 2][:], func=AF.Sigmoid, bias=lrup[:, 24 + c:25 + c]),
                                 reads=[BK(b0 + 2), "lrup"], writes=[N("i_")])
                            yield
                            S.op("act", lambda e: e.activation(out=a_[:], in_=r_[:], func=AF.Exp, scale=lrup[:, 32 + c:33 + c]),
                                 reads=[N("r_"), "lrup"], writes=[N("a_")])
                            yield
                            S.op("act", lambda e: e.activation(out=u_[:], in_=r_[:], func=AF.Exp, scale=lrup[:, 36 + c:37 + c]),
                                 reads=[N("r_"), "lrup"], writes=[N("u_")])
                            yield
                            S.op("dve", lambda e: e.tensor_scalar(out=u_[:], in0=u_[:], scalar1=1.0, scalar2=None, op0=ALU.min),
                                 reads=[N("u_")], writes=[N("u_")])
                            yield
                            S.op("act", lambda e: e.activation(out=u_[:], in_=u_[:], func=AF.Sqrt, scale=-1.0, bias=1.0),
                                 reads=[N("u_")], writes=[N("u_")])
                            yield
                            S.op("dve", lambda e: e.tensor_tensor(out=i_[:], in0=i_[:], in1=xb_[:], op=ALU.mult),
                                 reads=[N("i_"), N("xb")], writes=[N("i_")])
                            yield
                            S.op("dve", lambda e: e.tensor_tensor(out=u_[:], in0=u_[:], in1=i_[:], op=ALU.mult),
                                 reads=[N("i_"), N("u_")], writes=[N("u_")])
                            yield
                            init = 0.0 if g == 0 else hh[1 - sl][:, 511:512]
                            S.op("dve", lambda e: e.tensor_tensor_scan(out=hh[sl][:], data0=a_[:], data1=u_[:], initial=init,
                                                                       op0=ALU.mult, op1=ALU.add),
                                 reads=[N("a_"), N("u_"), N(f"hh{1 - sl}")], writes=[N(f"hh{sl}")])
                            yield
                            proj(2048 + c * 128, g, b0 + 3)
                            yield
                            S.op("act", lambda e: e.activation(out=gg[:], in_=banks[b0 + 3][:], func=AF.Gelu_apprx_tanh),
                                 reads=[BK(b0 + 3)], writes=[N("gg")])
                            yield
                            S.op("dve", lambda e: e.tensor_tensor(out=rec[sl][:], in0=hh[sl][:], in1=gg[:], op=ALU.mult),
                                 reads=[N(f"hh{sl}"), N("gg")], writes=[N(f"rec{sl}")])
                            S.dma("sp", f"rst{k}{sl}", lambda e: e.dma_start(out=am_w[:, 4 * g:4 * g + 4, 4 + c, :],
                                                                             in_=rec[sl][:].rearrange("p (q j) -> p q j", q=4)),
                                  reads=[N(f"rec{sl}")], writes=[("rec_s", c)])
                            yield

                    for c0 in (0, 2):
                        ga, gb = lru_chain(c0, 0), lru_chain(c0 + 1, 1)
                        alive = [ga, gb]
                        while alive:
                            for gen in list(alive):
                                if next(gen, "done") == "done":
                                    alive.remove(gen)
                    S.barrier()

            stop_at("C")
          with ExitStack() as ph:
              woutb = T(ph, "woutb", [128, 8, D], BF16)
              wpqb = T(ph, "wpqb", [128, 8, D], BF16)
              skb = T(ph, "skb", [128, 8, 128], BF16)
              with ExitStack() as ph2:
                  wst = [T(ph2, f"wst2{i}", [128, D]) for i in range(2)]
                  skf = T(ph2, "skf", [128, 8 * 128])
                  n = 0
                  for (src, dstt, scaled) in ((w_out, woutb, True), (w_pq, wpqb, False)):
                      for kc in range(8):
                          sl = n % 2
                          n += 1
                          S.dma("sp", f"wst2{sl}", lambda e: e.dma_start(out=wst[sl][:], in_=src[kc * 128:(kc + 1) * 128, :]),
                                writes=[f"wst2{sl}"])
                          if scaled:
                              S.op("dve", lambda e: e.tensor_scalar(out=dstt[:, kc, :], in0=wst[sl][:], scalar1=gout[:, kc:kc + 1],
                                                                    scalar2=None, op0=ALU.mult),
                                   reads=[f"wst2{sl}", "gout"], writes=["wb"])
                          else:
                              S.op("pool", lambda e: e.tensor_copy(out=dstt[:, kc, :], in_=wst[sl][:]), reads=[f"wst2{sl}"], writes=["wb"])
                  S.dma("sp", "skf", lambda e: e.dma_start(out=skf[:], in_=skT_d), writes=["skf"])
                  S.op("dve", lambda e: e.tensor_copy(out=skb[:], in_=skf[:].rearrange("p (h n) -> p h n", h=8)), reads=["skf"], writes=["wb"])
                  S.barrier()

              xt = T(ph, "xt_e", [128, D])
              am = T(ph, "am", [128, 8, 128])
              amb = T(ph, "amb", [128, 8, 128], BF16)
              st = T(ph, "st_e", [128, 8])
              t1 = T(ph, "t1", [128, D])
              amq = t1[:].rearrange("p (k j) -> p k j", k=8)
              h2bs = [T(ph, f"h2b{i}", [128, D], BF16) for i in range(3)]
              junk = T(ph, "junk_e", [128, D], BF16)
              h2T = T(ph, "h2T", [128, 8, 128], BF16)
              qpT = T(ph, "qpT", [128, 8, 128], BF16)
              big8 = T(ph, "big8", [128, 2048])
              scr = big8[:].rearrange("p (h n) -> p h n", h=16)
              cand = big8[:].rearrange("p (h n) -> p h n", h=8)
              scws = [T(ph, f"scw{i}", [128, 128]) for i in range(2)]
              v12 = [T(ph, f"v12_{i}", [128, 8, 16]) for i in range(2)]
              i12 = [T(ph, f"i12_{i}", [128, 8, 16], U32) for i in range(2)]
              i12f = [T(ph, f"i12f_{i}", [128, 8, 16]) for i in range(2)]
              candws = [T(ph, f"candw{i}", [128, 256]) for i in range(2)]
              tv = T(ph, "tv", [128, 8, 16])
              tp = T(ph, "tp", [128, 8, 16], U32)
              ab = [T(ph, f"ab{i}", [128, 8, 16], U32) for i in range(2)]
              abf = [T(ph, f"abf{i}", [128, 8, 16]) for i in range(2)]
              isel = [T(ph, f"isel{i}", [128, 8, 16]) for i in range(2)]
              eidf = T(ph, "eidf", [128, 128])
              gs8 = T(ph, "gs8", [128, 8])
              x1s = [T(ph, f"x1_{i}", [128, D]) for i in range(3)]
              h2s = [T(ph, f"h2_{i}", [128, D]) for i in range(3)]
              eidxs = [T(ph, f"eidx{i}", [128, 128], I32) for i in range(3)]
              gsms = [T(ph, f"gsm{i}", [128, 8, 16]) for i in range(3)]
              NSL = 7
              uv = [T(ph, f"uv{i}", [128, GP, 2 * D], BF16) for i in range(NSL)]
              actp = T(ph, "actp", [128, 128])
              actg = T(ph, "actg", [128, 128])
              coef = T(ph, "coef", [128, 128])
              dg = [T(ph, f"dg{i}", [128, 128], BF16) for i in range(4)]
              junk2 = T(ph, "junk2", [128, D], BF16)
              junk3 = T(ph, "junk3", [128, D], BF16)
              NPR = 3
              prod = [T(ph, f"prod{i}", [128, D], BF16) for i in range(NPR)]
              ot = T(ph, "ot", [128, D])

              def X(tt):
                  par = tt % 3
                  x1, h2, eidx, gsm = x1s[par], h2s[par], eidxs[par], gsms[par]
                  h2b = h2bs[par]
                  RX1, RH2, REI, RGS = ("x1", par), ("h2", par), ("eidx", par), ("gsm", par)
                  RHB = ("h2b", par)
                  tok = slice(tt * 128, (tt + 1) * 128)
                  S.dma("sp", "xe", lambda e: e.dma_start(out=xt[:], in_=x[b, tok, :]), writes=["xt_e"])
                  S.dma("sp", "am_a", lambda e: e.dma_start(out=am[:], in_=am_s[tok, :].rearrange("p (k j) -> p k j", k=8)),
                        reads=["attn_s"] + [("rec_s", q) for q in range(4)], writes=["am_a", "am_r"])
                  yield
                  S.op("act", lambda e: e.activation(out=amb[:], in_=am[:], func=AF.Identity), reads=["am_a", "am_r"], writes=["amb"])
                  S.op("act", lambda e: e.activation(out=amq, in_=am[:], func=AF.Square), reads=["am_a", "am_r"], writes=["t1"])
                  yield
                  for kc in range(8):
                      hf = kc // 4
                      S.op("pe", lambda e: e.matmul(banks[3][:, hf * 2:hf * 2 + 2], lhsT=amq[:, kc, :], rhs=cst[:, 256:258],
                                                    start=(kc % 4 == 0), stop=(kc % 4 == 3)),
                           reads=["t1", "cst"], writes=[BK(3)])
                  yield
                  S.op("act", lambda e: e.activation(out=st[:, 0:4], in_=banks[3][:, 0:4], func=AF.Sqrt, scale=-1.0 / 512, bias=EPS),
                       reads=[BK(3)], writes=["st_e"])
                  S.op("dve", lambda e: e.reciprocal(out=st[:, 0:4], in_=st[:, 0:4]), reads=["st_e"], writes=["st_e"])
                  yield
                  for nh in range(2):
                      for part in range(2):
                          bk = nh * 2 + part
                          for k4 in range(4):
                              kc = part * 4 + k4
                              S.op("pe", lambda e: e.matmul(banks[bk][:], lhsT=amb[:, kc, :], rhs=woutb[:, kc, nh * 512:(nh + 1) * 512],
                                                            start=(k4 == 0), stop=(k4 == 3)),
                                   reads=["amb", "wb"], writes=[BK(bk)])
                          yield
                      cs = slice(nh * 512, (nh + 1) * 512)
                      S.op("act", lambda e: e.activation(out=t1[:, cs], in_=banks[nh * 2][:], func=AF.Copy, scale=st[:, 0:1]),
                           reads=[BK(nh * 2), "st_e"], writes=["t1"])
                      S.op("dve", lambda e: e.scalar_tensor_tensor(out=t1[:, cs], in0=banks[nh * 2 + 1][:], scalar=st[:, 2:3], in1=t1[:, cs],
                                                                   op0=ALU.mult, op1=ALU.add),
                           reads=[BK(nh * 2 + 1), "st_e", "t1"], writes=["t1"])
                      yield
                  S.op("dve", lambda e: e.tensor_tensor(out=t1[:], in0=t1[:], in1=mod[:, 0:1024], op=ALU.mult),
                       reads=["t1", "mod"], writes=["t1"])
                  S.op("dve", lambda e: e.tensor_tensor(out=x1[:], in0=t1[:], in1=xt[:], op=ALU.add),
                       reads=["t1", "xt_e"], writes=[RX1])
                  yield
                  if dbg:
                      S.dma("sp", "dbg", lambda e: e.dma_start(out=dbg_x1[tok, :], in_=x1[:]), reads=[RX1], writes=["dbg_x1"])
                  S.op("act", lambda e: e.activation(out=junk[:], in_=x1[:], func=AF.Square, accum_out=st[:, 4:5]),
                       reads=[RX1], writes=["junk_e", "st_e2"])
                  S.op("act", lambda e: e.activation(out=st[:, 5:6], in_=st[:, 4:5], func=AF.Sqrt, scale=1.0 / D, bias=EPS),
                       reads=["st_e2"], writes=["st_e2"])
                  S.op("dve", lambda e: e.reciprocal(out=st[:, 6:7], in_=st[:, 5:6]), reads=["st_e2"], writes=["st_e2"])
                  yield
                  S.op("dve", lambda e: e.scalar_tensor_tensor(out=h2[:], in0=x1[:], scalar=st[:, 6:7], in1=mod[:, 2048:3072],
                                                               op0=ALU.mult, op1=ALU.mult),
                       reads=[RX1, "st_e2", "mod"], writes=[RH2])
                  S.op("dve", lambda e: e.tensor_tensor(out=h2[:], in0=h2[:], in1=mod[:, 1024:2048], op=ALU.add),
                       reads=[RH2, "mod"], writes=[RH2])
                  S.op("act", lambda e: e.activation(out=h2b[:], in_=h2[:], func=AF.Identity), reads=[RH2], writes=[RHB])
                  yield
                  pv = banks[0][:].bitcast(BF16)
                  for kc in range(8):
                      S.op("pe", lambda e: e.transpose(out=pv[:, kc * 128:(kc + 1) * 128], in_=h2b[:, kc * 128:(kc + 1) * 128],
                                                       identity=identb[:]),
                           reads=[RHB, "identb"], writes=[BK(0)])
                  S.op("act", lambda e: e.activation(out=h2T[:], in_=pv.rearrange("p (k j) -> p k j", k=8), func=AF.Identity),
                       reads=[BK(0)], writes=["h2T"])
                  yield
                  for hd in range(8):
                      bk = 2 + hd // 4
                      for kc in range(8):
                          S.op("pe", lambda e: e.matmul(banks[bk][:, (hd % 4) * 128:(hd % 4 + 1) * 128], lhsT=wpqb[:, kc, hd * 128:(hd + 1) * 128],
                                                        rhs=h2T[:, kc, :], start=(kc == 0), stop=(kc == 7)),
                               reads=["wb", "h2T"], writes=[BK(bk)])
                      yield
                  for hf in range(2):
                      S.op("act", lambda e: e.activation(out=qpT[:, hf * 4:(hf + 1) * 4, :],
                                                         in_=banks[2 + hf][:].rearrange("p (h j) -> p h j", h=4), func=AF.Identity),
                           reads=[BK(2 + hf)], writes=["qpT"])
                  yield
                  for hd in range(8):
                      for half in range(2):
                          hh_ = half * 8 + hd
                          bk = hh_ // 4
                          pb = 64 * half
                          S.op("pe", lambda e: e.matmul(banks[bk][:, (hh_ % 4) * 128:(hh_ % 4 + 1) * 128], lhsT=qpT[pb:pb + 64, hd, :],
                                                        rhs=skb[pb:pb + 64, hd, :], start=True, stop=True),
                               reads=["qpT", "wb"], writes=[BK(bk)])
                  yield
                  for q in range(4):
                      S.op("act", lambda e: e.activation(out=scr[:, q * 4:(q + 1) * 4, :], in_=banks[q][:].rearrange("p (h j) -> p h j", h=4),
                                                         func=AF.Identity), reads=[BK(q)], writes=["big8"])
                  yield
                  for hd in range(8):
                      ch = [(v12[hf], i12[hf], scr[:, hf * 8 + hd, :], scws[hf], hf) for hf in range(2)]
                      for (vv, ii, src, sw, hf) in ch:
                          S.op("dve", lambda e: e.max(out=vv[:, hd, 0:8], in_=src), reads=["big8"], writes=[("v12", hf)])
                      for (vv, ii, src, sw, hf) in ch:
                          S.op("dve", lambda e: e.max_index(out=ii[:, hd, 0:8], in_max=vv[:, hd, 0:8], in_values=src),
                               reads=["big8", ("v12", hf)], writes=[("i12", hf)])
                      for (vv, ii, src, sw, hf) in ch:
                          S.op("dve", lambda e: e.match_replace(out=sw[:], in_to_replace=vv[:, hd, 0:8], in_values=src, imm_value=-1e30),
                               reads=["big8", ("v12", hf)], writes=[("scw", hf)])
                      yield
                      for (vv, ii, src, sw, hf) in ch:
                          S.op("dve", lambda e: e.max(out=vv[:, hd, 8:16], in_=sw[:]), reads=[("scw", hf)], writes=[("v12", hf)])
                      for (vv, ii, src, sw, hf) in ch:
                          S.op("dve", lambda e: e.max_index(out=ii[:, hd, 8:16], in_max=vv[:, hd, 8:16], in_values=sw[:]),
                               reads=[("scw", hf), ("v12", hf)], writes=[("i12", hf)])
                      yield
                  for half in range(2):
                      S.op("dve", lambda e: e.tensor_copy(out=i12f[half][:], in_=i12[half][:]), reads=[("i12", half)], writes=["i12f"])
                  c4 = cand.rearrange("p h (a b) -> p h a b", a=16)
                  S.op("dve", lambda e: e.tensor_tensor(out=c4, in0=v12[0][:].unsqueeze(3).to_broadcast([128, 8, 16, 16]),
                                                        in1=v12[1][:].unsqueeze(2).to_broadcast([128, 8, 16, 16]), op=ALU.add),
                       reads=[("v12", 0), ("v12", 1)], writes=["big8"])
                  yield
                  for h0 in range(0, 8, 2):
                      ch = [(h0 + q, cand[:, h0 + q, :], candws[q], q) for q in range(2)]
                      for (hd, src, cw, q) in ch:
                          S.op("dve", lambda e: e.max(out=tv[:, hd, 0:8], in_=src), reads=["big8"], writes=[("tv", q)])
                      for (hd, src, cw, q) in ch:
                          S.op("dve", lambda e: e.max_index(out=tp[:, hd, 0:8], in_max=tv[:, hd, 0:8], in_values=src),
                               reads=["big8", ("tv", q)], writes=[("tp", q)])
                      for (hd, src, cw, q) in ch:
                          S.op("dve", lambda e: e.match_replace(out=cw[:], in_to_replace=tv[:, hd, 0:8], in_values=src, imm_value=-1e30),
                               reads=["big8", ("tv", q)], writes=[("candw", q)])
                      yield
                      for (hd, src, cw, q) in ch:
                          S.op("dve", lambda e: e.max(out=tv[:, hd, 8:16], in_=cw[:]), reads=[("candw", q)], writes=[("tv", q)])
                      for (hd, src, cw, q) in ch:
                          S.op("dve", lambda e: e.max_index(out=tp[:, hd, 8:16], in_max=tv[:, hd, 8:16], in_values=cw[:]),
                               reads=[("candw", q), ("tv", q)], writes=[("tp", q)])
                      yield
                  S.op("dve", lambda e: e.tensor_single_scalar(out=ab[0][:], in_=tp[:], scalar=4, op=ALU.logical_shift_right), reads=[("tp", 0), ("tp", 1)], writes=["ab"])
                  S.op("dve", lambda e: e.tensor_single_scalar(out=ab[1][:], in_=tp[:], scalar=15, op=ALU.bitwise_and), reads=[("tp", 0), ("tp", 1)], writes=["ab"])
                  yield
                  e4 = cand.rearrange("p h (k a) -> p h k a", k=16)
                  io4 = iota_ka.rearrange("p (k a) -> p k a", k=16).unsqueeze(1).to_broadcast([128, 8, 16, 16])
                  for half in range(2):
                      S.op("dve", lambda e: e.tensor_copy(out=abf[half][:], in_=ab[half][:]), reads=["ab"], writes=["abf"])
                      S.op("dve", lambda e: e.tensor_tensor(out=e4, in0=abf[half][:].unsqueeze(3).to_broadcast([128, 8, 16, 16]), in1=io4, op=ALU.is_equal),
                           reads=["abf", "cst"], writes=["big8"])
                      yield
                      S.op("dve", lambda e: e.tensor_tensor(out=e4, in0=e4, in1=i12f[half][:].unsqueeze(2).to_broadcast([128, 8, 16, 16]), op=ALU.mult),
                           reads=["big8", "i12f"], writes=["big8"])
                      yield
                      S.op("dve", lambda e: e.reduce_sum(out=isel[half][:], in_=e4, axis=AX.X), reads=["big8"], writes=["isel"])
                      yield
                  S.op("dve", lambda e: e.scalar_tensor_tensor(out=eidf[:].rearrange("p (h k) -> p h k", h=8), in0=isel[0][:], scalar=128.0,
                                                               in1=isel[1][:], op0=ALU.mult, op1=ALU.add),
                       reads=["isel"], writes=["eidf"])
                  S.op("dve", lambda e: e.tensor_scalar(out=eidf[:], in0=eidf[:], scalar1=0.0, scalar2=16383.0, op0=ALU.max, op1=ALU.min),
                       reads=["eidf"], writes=["eidf"])
                  S.op("dve", lambda e: e.tensor_copy(out=eidx[:], in_=eidf[:]), reads=["eidf"], writes=[REI])
                  yield
                  S.op("dve", lambda e: e.tensor_tensor(out=gsm[:], in0=tv[:], in1=tv[:, :, 0:1].to_broadcast([128, 8, 16]), op=ALU.subtract),
                       reads=[("tv", 0), ("tv", 1)], writes=[RGS])
                  S.op("act", lambda e: e.activation(out=gsm[:], in_=gsm[:], func=AF.Exp), reads=[RGS], writes=[RGS])
                  S.op("dve", lambda e: e.reduce_sum(out=gs8[:], in_=gsm[:], axis=AX.X), reads=[RGS], writes=["gs8"])
                  S.op("dve", lambda e: e.reciprocal(out=gs8[:], in_=gs8[:]), reads=["gs8"], writes=["gs8"])
                  S.op("dve", lambda e: e.tensor_tensor(out=gsm[:], in0=gsm[:], in1=gs8[:].unsqueeze(2).to_broadcast([128, 8, 16]), op=ALU.mult),
                       reads=[RGS, "gs8"], writes=[RGS])
                  yield

              NGRP = 128 // GP
              slot_free = {}
              LAG = NSL - 2
              XYIELDS = 59

              NST = 3

              def issue(G):
                  tt, gi = divmod(G, NGRP)
                  par = tt % NST
                  eidx = eidxs[par]
                  sl = G % NSL
                  for p in range(GP):
                      j = gi * GP + p
                      S.dma("pool", f"g{sl}_{p}", lambda e: e.indirect_dma_start(out=uv[sl][:, p, :], out_offset=None, in_=uvb,
                                                                                 in_offset=bass.IndirectOffsetOnAxis(ap=eidx[:, j:j + 1], axis=0)),
                            reads=[("eidx", par), "uvb"], writes=[(f"uv{sl}", p)])

              def consume_a(G):
                  tt, gi = divmod(G, NGRP)
                  par = tt % NST
                  h2, h2b = h2s[par], h2bs[par]
                  sl = G % NSL
                  cs = slice(gi * GP, (gi + 1) * GP)
                  uvr = [(f"uv{sl}", q) for q in range(GP)]
                  for p in range(GP):
                      j = gi * GP + p
                      if p % 2 == 0:
                          S.op("dve", lambda e: e.scalar_tensor_tensor(out=junk2[:], in0=uv[sl][:, p, 0:D], scalar=1.0, in1=h2[:],
                                                                       op0=ALU.mult, op1=ALU.mult, accum_out=actp[:, j:j + 1]),
                               reads=uvr + [("h2", par)], writes=["junk2", ("actp", gi)])
                      else:
                          ps_ = G % NPR
                          S.op("dve", lambda e: e.tensor_tensor(out=prod[ps_][:], in0=uv[sl][:, p, 0:D], in1=h2b[:], op=ALU.mult),
                               reads=uvr + [("h2b", par)], writes=[f"prod{ps_}"])
                          S.op("act", lambda e: e.activation(out=junk3[:], in_=prod[ps_][:], func=AF.Identity, accum_out=actp[:, j:j + 1]),
                               reads=[f"prod{ps_}"], writes=["junk3", ("actp", gi)])
                  S.op("act", lambda e: e.activation(out=actg[:, cs], in_=actp[:, cs], func=AF.Gelu_apprx_tanh),
                       reads=[("actp", gi)], writes=[("actg", gi)])

              def consume_b(G):
                  tt, gi = divmod(G, NGRP)
                  par = tt % NST
                  gsm2 = gsms[par][:].rearrange("p h k -> p (h k)")
                  sl = G % NSL
                  cs = slice(gi * GP, (gi + 1) * GP)
                  uvr = [(f"uv{sl}", q) for q in range(GP)]
                  ab0 = 4 + 2 * (tt % 2)
                  S.op("dve", lambda e: e.tensor_tensor(out=coef[:, cs], in0=actg[:, cs], in1=gsm2[:, cs], op=ALU.mult),
                       reads=[("actg", gi), ("gsm", par)], writes=[("coef", gi)])
                  for p in range(GP):
                      j = gi * GP + p
                      ds = j % 4
                      S.op("act", lambda e: e.activation(out=dg[ds][:], in_=identb[:], func=AF.Copy, scale=coef[:, j:j + 1]),
                           reads=["identb", ("coef", gi)], writes=[f"dg{ds}"])
                      for nh in range(2):
                          S.op("pe", lambda e: e.matmul(banks[ab0 + nh][:], lhsT=dg[ds][:], rhs=uv[sl][:, p, D + nh * 512:D + (nh + 1) * 512],
                                                        start=(j == 0), stop=(j == 127)),
                               reads=[f"dg{ds}"] + uvr, writes=[BK(ab0 + nh)])
                  if gi == NGRP - 1:
                      x1 = x1s[par]
                      tok = slice(tt * 128, (tt + 1) * 128)
                      for nh in range(2):
                          cs2 = slice(nh * 512, (nh + 1) * 512)
                          S.op("dve", lambda e: e.tensor_tensor(out=ot[:, cs2], in0=banks[ab0 + nh][:],
                                                                in1=mod[:, 3072 + nh * 512:3072 + (nh + 1) * 512], op=ALU.mult),
                               reads=[BK(ab0 + nh), "mod"], writes=["ot"])
                      S.op("dve", lambda e: e.tensor_tensor(out=ot[:], in0=ot[:], in1=x1[:], op=ALU.add), reads=["ot", ("x1", par)], writes=["ot"])
                      S.dma("sp", "ost", lambda e: e.dma_start(out=out[b, tok, :], in_=ot[:]), reads=["ot"], writes=["out"])

              nyield = 0
              for _ in X(0):
                  nyield += 1
              assert nyield == XYIELDS, nyield
              NTOT = nt_de * NGRP
              xg = None
              xdone = 0
              XSPAN = NGRP - LAG - 3
              for G in range(NTOT + LAG + 1):
                  if G < NTOT:
                      issue(G)
                  if 0 <= G - LAG < NTOT:
                      consume_a(G - LAG)
                  if 0 <= G - LAG - 1 < NTOT:
                      consume_b(G - LAG - 1)
                  tcur, k = divmod(G, NGRP)
                  if k == 0 and tcur + 1 < nt_de:
                      xg = X(tcur + 1)
                      xdone = 0
                  if xg is not None:
                      tgt = min(XYIELDS, ((k + 1) * XYIELDS) // XSPAN + 1)
                      while xdone < tgt:
                          if next(xg, "done") == "done":
                              xg = None
                              break
                          xdone += 1
              S.barrier()
        S.barrier(["sp"])
    return nc


def _consts():
    c = np.zeros((128, NCONST), np.float32)
    c[:, 0:128] = np.eye(128, dtype=np.float32)
    jj = np.arange(128)[:, None]
    ss = np.arange(128)[None, :]
    c[:, 128:256] = -(jj >= ss).astype(np.float32)
    c[:, 256:384] = -1.0
    c[:, 384:512] = ((jj // 64) == (ss // 64)).astype(np.float32)
    c[:, 512:768] = np.tile(np.arange(16, dtype=np.float32), 16)[None, :]
    return c


def _masks():
    m = np.zeros((128, 2048), np.float32)
    jj = np.arange(128)[:, None]
    t = np.arange(512)[None, :]
    for j in range(4):
        m[:, j * 512:(j + 1) * 512] = ((128 * j + jj) < t).astype(np.float32)
    return m


def _prep(inputs):
    f = lambda k: np.ascontiguousarray(np.asarray(inputs[k], dtype=np.float32))
    x = f("x"); c = f("c")
    shared = {
        "w_mod": f("w_mod")[0], "b_mod": f("b_mod"), "g1": f("g_norm1"), "g2": f("g_norm2"),
        "w_in": f("w_in")[0], "w_out": f("w_out")[0], "w_pq": f("w_pq")[0],
        "expert_u": f("expert_u")[0], "expert_v": f("expert_v")[0], "consts": _consts(), "masks": _masks(),
    }
    gq = f("g_q")[0]; gk = f("g_k")[0]
    shared["gqk"] = np.ascontiguousarray(np.stack([np.tile(gq, 2), np.tile(gk, 2)], axis=1))
    lr = np.zeros((128, 32), np.float32)
    cw = f("conv_w")[0]
    for cch in range(4):
        for k in range(4):
            lr[:, cch * 4 + k] = cw[k, cch * 128:(cch + 1) * 128]
    lr[:, 16:20] = f("conv_b")[0].reshape(4, 128).T
    lr[:, 20:24] = f("b_rg")[0].reshape(4, 128).T
    lr[:, 24:28] = f("b_ig")[0].reshape(4, 128).T
    lr[:, 28:32] = f("lru_lambda")[0].reshape(4, 128).T
    shared["lrup"] = lr
    wg = np.zeros((128, 8, 128), np.float32)
    for gi, key in enumerate(("w_rg", "w_ig")):
        w = f(key)[0]
        for cch in range(4):
            wg[0:64, gi * 4 + cch, 0:64] = w[2 * cch]
            wg[64:128, gi * 4 + cch, 64:128] = w[2 * cch + 1]
    shared["wg_bd"] = wg.reshape(128, 1024)
    go = np.concatenate([f("g_out_attn")[0], f("g_out_lru")[0]])
    shared["gout"] = np.ascontiguousarray(go.reshape(8, 128).T)
    sk1 = f("sub_keys1")[0]; sk2 = f("sub_keys2")[0]
    sk = np.concatenate([sk1.transpose(2, 0, 1), sk2.transpose(2, 0, 1)], axis=0)
    shared["skT"] = np.ascontiguousarray(sk.reshape(128, 1024))
    maps = []
    for i in range(NCORES):
        m = dict(shared)
        m["x"] = np.ascontiguousarray(x[2 * i:2 * i + 2])
        cc = c[2 * i:2 * i + 2]
        m["cT"] = np.ascontiguousarray(cc.reshape(2, 8, 128).transpose(2, 1, 0).reshape(128, 16))
        maps.append(m)
    return maps


def kernel(**inputs):
    maps = _prep(inputs)
    nc = build_nc()
    res = run_bass_kernel_spmd(nc, maps, core_ids=list(range(NCORES)))
    outs = [np.asarray(r["out"]) for r in res.results]
    return np.concatenate(outs, axis=0).astype(np.float32)
```
